# Optimizing a Trainium2 kernel written in Bass

```python
import math
import jax, jax.numpy as jnp
from jax import lax
import numpy as np

D_MODEL = 1024
BATCH = 16
SEQ = 2048
DEPTH = 1

CTX_LEN = 256
GRID_W = 64

SSD_HEADS = 16
SSD_HEAD_DIM = 64
D_SSD = SSD_HEADS * SSD_HEAD_DIM
SSD_GROUPS = 2
SSD_HEADS_PER_GROUP = SSD_HEADS // SSD_GROUPS
D_STATE = 128
CONV_W = 5
CHUNK = 128
D_XBC = D_SSD + 2 * SSD_GROUPS * D_STATE

ATTN_Q_HEADS = 16
ATTN_KV_HEADS = 4
HEAD_DIM = 64
GQA_GROUP = ATTN_Q_HEADS // ATTN_KV_HEADS
D_ATTN = ATTN_Q_HEADS * HEAD_DIM
ROPE_THETA = 10000.0
Q_BLOCK = 128

D_MIX = D_SSD + D_ATTN

OFF_Z = 0
OFF_Q = OFF_Z + D_SSD
OFF_XBC = OFF_Q + D_ATTN
OFF_DT = OFF_XBC + D_XBC
OFF_K = OFF_DT + 2 * SSD_HEADS
OFF_V = OFF_K + ATTN_KV_HEADS * HEAD_DIM
D_IN_PROJ = OFF_V + ATTN_KV_HEADS * HEAD_DIM

MOE_GROUPS = 4
EXPERTS_PER_GROUP = 4
N_EXPERTS = MOE_GROUPS * EXPERTS_PER_GROUP
TOP_K_IN_GROUP = 2
D_EXPERT = 256

kernel_name = 'hybrid_ssd_gqa_hmoe_block'

F32 = jnp.float32


def layer_norm(x, g, b, eps=1e-5):
    xf = x.astype(F32)
    mu = jnp.mean(xf, -1, keepdims=True)
    var = jnp.mean(jnp.square(xf - mu), -1, keepdims=True)
    return ((xf - mu) * lax.rsqrt(var + eps) * g + b).astype(x.dtype)


def rms_norm(x, g, eps=1e-6):
    xf = x.astype(F32)
    return (xf * lax.rsqrt(jnp.mean(xf * xf, -1, keepdims=True) + eps) * g).astype(x.dtype)


def dwconv_silu(u, w, b):
    y = lax.conv_general_dilated(u, w[:, None, :], (1,), [(CONV_W // 2, CONV_W // 2)],
                                 dimension_numbers=('NWC', 'WIO', 'NWC'),
                                 feature_group_count=u.shape[-1])
    return jax.nn.silu(y + b)


def axial_rope_tables(n_rows):
    rows = jnp.broadcast_to(jnp.arange(n_rows, dtype=F32)[:, None], (n_rows, GRID_W)).reshape(-1)
    cols = jnp.broadcast_to(jnp.arange(GRID_W, dtype=F32)[None, :], (n_rows, GRID_W)).reshape(-1)
    axis_dim = HEAD_DIM // 2
    inv_freq = jnp.power(ROPE_THETA, -jnp.arange(0, axis_dim, 2, dtype=F32) / axis_dim)
    ang = jnp.concatenate([rows[:, None] * inv_freq, cols[:, None] * inv_freq], -1)
    return jnp.cos(ang), jnp.sin(ang)


def apply_rope(x, cos, sin):
    xf = x.astype(F32).reshape(*x.shape[:-1], HEAD_DIM // 2, 2)
    x0, x1 = xf[..., 0], xf[..., 1]
    c = cos[None, :, None, :]
    s = sin[None, :, None, :]
    out = jnp.stack([x0 * c - x1 * s, x0 * s + x1 * c], -1)
    return out.reshape(x.shape).astype(x.dtype)


def ssd_scan(xs, dt, A, Bm, Cm, h0, with_output):
    b, L = xs.shape[:2]
    nc = L // CHUNK
    G, R, P, N = SSD_GROUPS, SSD_HEADS_PER_GROUP, SSD_HEAD_DIM, D_STATE
    xdt = (xs.astype(F32) * dt[..., None]).reshape(b, nc, CHUNK, G, R, P)
    a_cs = jnp.cumsum((dt * A).reshape(b, nc, CHUNK, G, R), axis=2)
    Bc = Bm.astype(F32).reshape(b, nc, CHUNK, G, N)
    w_state = jnp.exp(a_cs[:, :, -1:] - a_cs)
    chunk_states = jnp.einsum('bcsgn,bcsgr,bcsgrp->bcgrpn', Bc, w_state, xdt)
    chunk_decay = jnp.exp(a_cs[:, :, -1])

    def carry_state(h, inp):
        s_c, d_c = inp
        return h * d_c[..., None, None] + s_c, h

    h_final, h_in = lax.scan(carry_state, h0,
                             (jnp.moveaxis(chunk_states, 1, 0), jnp.moveaxis(chunk_decay, 1, 0)))
    if not with_output:
        return h_final
    Cc = Cm.astype(F32).reshape(b, nc, CHUNK, G, N)
    diff = a_cs[:, :, :, None] - a_cs[:, :, None]
    lower = jnp.tril(jnp.ones((CHUNK, CHUNK), bool))[:, :, None, None]
    decay = jnp.where(lower, jnp.exp(jnp.where(lower, diff, 0.0)), 0.0)
    cb = jnp.einsum('bcqgn,bcsgn->bcqsg', Cc, Bc)
    y_diag = jnp.einsum('bcqsgr,bcsgrp->bcqgrp', cb[..., None] * decay, xdt)
    y_off = jnp.einsum('bcqgn,bcgrpn,bcqgr->bcqgrp', Cc, jnp.moveaxis(h_in, 0, 1), jnp.exp(a_cs))
    return (y_diag + y_off).reshape(b, L, G, R, P), h_final


def ssd_bidirectional(xs, Bm, Cm, dt, A, h0_f, h0_b, with_output):
    fl = lambda u: jnp.flip(u, axis=1)
    out_f = ssd_scan(xs, dt[:, :, 0], A[0], Bm, Cm, h0_f, with_output)
    out_b = ssd_scan(fl(xs), fl(dt[:, :, 1]), A[1], fl(Bm), fl(Cm), h0_b, with_output)
    return out_f, out_b


def ssd_prepare(xbc_raw, dt_raw, p):
    b, L, _ = xbc_raw.shape
    xbc = dwconv_silu(xbc_raw, p['conv_w'], p['conv_b'])
    gn = SSD_GROUPS * D_STATE
    xs = xbc[..., :D_SSD].reshape(b, L, SSD_GROUPS, SSD_HEADS_PER_GROUP, SSD_HEAD_DIM)
    Bm = xbc[..., D_SSD:D_SSD + gn].reshape(b, L, SSD_GROUPS, D_STATE)
    Cm = xbc[..., D_SSD + gn:].reshape(b, L, SSD_GROUPS, D_STATE)
    dt = jax.nn.softplus(dt_raw.astype(F32).reshape(b, L, 2, SSD_HEADS) + p['dt_bias'].astype(F32))
    dt = dt.reshape(b, L, 2, SSD_GROUPS, SSD_HEADS_PER_GROUP)
    A = -jnp.exp(p['a_log'].astype(F32)).reshape(2, SSD_GROUPS, SSD_HEADS_PER_GROUP)
    return xs, Bm, Cm, dt, A


def ssd_output(y_f, y_b, xs, z, p):
    b, L = z.shape[:2]
    skip = p['d_skip'].astype(F32).reshape(SSD_GROUPS, SSD_HEADS_PER_GROUP)[..., None]
    y = y_f + jnp.flip(y_b, axis=1) + xs.astype(F32) * skip
    y = y.reshape(b, L, D_SSD) * jax.nn.silu(z.astype(F32))
    yg = y.reshape(b, L, SSD_GROUPS, D_SSD // SSD_GROUPS)
    yg = yg * lax.rsqrt(jnp.mean(yg * yg, -1, keepdims=True) + 1e-6)
    return (yg.reshape(b, L, D_SSD) * p['ssd_norm_g']).astype(z.dtype)


def gqa_block_attention(q, k, v):
    b, L = q.shape[:2]
    nblk = L // Q_BLOCK
    qb = q.reshape(b, nblk, Q_BLOCK, ATTN_KV_HEADS, GQA_GROUP, HEAD_DIM).swapaxes(0, 1)
    scale = HEAD_DIM ** -0.5

    def one_block(q_blk):
        s = jnp.einsum('bqhgd,bkhd->bhgqk', q_blk, k, preferred_element_type=F32) * scale
        pr = jax.nn.softmax(s, axis=-1).astype(v.dtype)
        return jnp.einsum('bhgqk,bkhd->bqhgd', pr, v)

    o = lax.map(one_block, qb)
    return o.swapaxes(0, 1).reshape(b, L, D_ATTN)


def context_mixer(hc, p, with_output):
    b, S, _ = hc.shape
    side = hc @ p['w_in'][:, OFF_XBC:]
    xbc_raw = side[..., :OFF_DT - OFF_XBC]
    dt_raw = side[..., OFF_DT - OFF_XBC:OFF_K - OFF_XBC]
    k = rms_norm(side[..., OFF_K - OFF_XBC:OFF_V - OFF_XBC].reshape(b, S, ATTN_KV_HEADS, HEAD_DIM), p['k_norm_g'])
    v = side[..., OFF_V - OFF_XBC:].reshape(b, S, ATTN_KV_HEADS, HEAD_DIM)
    xs, Bm, Cm, dt, A = ssd_prepare(xbc_raw, dt_raw, p)
    h0 = jnp.zeros((b, SSD_GROUPS, SSD_HEADS_PER_GROUP, SSD_HEAD_DIM, D_STATE), F32)
    if not with_output:
        hf, hb = ssd_bidirectional(xs, Bm, Cm, dt, A, h0, h0, False)
        return (k, v, hf, hb), None
    (y_f, hf), (y_b, hb) = ssd_bidirectional(xs, Bm, Cm, dt, A, h0, h0, True)
    zq = hc @ p['w_in'][:, :OFF_XBC]
    z = zq[..., OFF_Z:OFF_Q]
    q = rms_norm(zq[..., OFF_Q:].reshape(b, S, ATTN_Q_HEADS, HEAD_DIM), p['q_norm_g'])
    y_ssd = ssd_output(y_f, y_b, xs, z, p)
    o = gqa_block_attention(q, k, v)
    return (k, v, hf, hb), jnp.concatenate([y_ssd, o], -1) @ p['w_out']


def latent_mixer(hx, ctx_side, p, cos, sin):
    b, L, _ = hx.shape
    k_ctx, v_ctx, hf_ctx, hb_ctx = ctx_side
    proj = hx @ p['w_in']
    z = proj[..., OFF_Z:OFF_Q]
    q = proj[..., OFF_Q:OFF_XBC].reshape(b, L, ATTN_Q_HEADS, HEAD_DIM)
    xbc_raw = proj[..., OFF_XBC:OFF_DT]
    dt_raw = proj[..., OFF_DT:OFF_K]
    k = proj[..., OFF_K:OFF_V].reshape(b, L, ATTN_KV_HEADS, HEAD_DIM)
    v = proj[..., OFF_V:].reshape(b, L, ATTN_KV_HEADS, HEAD_DIM)
    xs, Bm, Cm, dt, A = ssd_prepare(xbc_raw, dt_raw, p)
    (y_f, _), (y_b, _) = ssd_bidirectional(xs, Bm, Cm, dt, A, hf_ctx, hb_ctx, True)
    y_ssd = ssd_output(y_f, y_b, xs, z, p)
    q = apply_rope(rms_norm(q, p['q_norm_g']), cos, sin)
    k = apply_rope(rms_norm(k, p['k_norm_g']), cos, sin)
    o = gqa_block_attention(q, jnp.concatenate([k_ctx, k], axis=1), jnp.concatenate([v_ctx, v], axis=1))
    return jnp.concatenate([y_ssd, o], -1) @ p['w_out']


def hier_moe(h, p):
    b, L, d = h.shape
    t = h.reshape(-1, d)
    g_prob = jax.nn.softmax((t @ p['w_rg'] + p['b_rg']).astype(F32), axis=-1)
    g_val, g_idx = lax.top_k(g_prob, 1)
    e_logits = (t @ p['w_re'] + p['b_re']).astype(F32).reshape(-1, MOE_GROUPS, EXPERTS_PER_GROUP)
    e_sel = jnp.take_along_axis(e_logits, g_idx[:, :, None], axis=1)[:, 0]
    e_val, e_idx = lax.top_k(jax.nn.softmax(e_sel, axis=-1), TOP_K_IN_GROUP)
    e_val = e_val / jnp.sum(e_val, -1, keepdims=True)
    w_group = jnp.sum(jax.nn.one_hot(e_idx, EXPERTS_PER_GROUP, dtype=F32) * e_val[..., None], axis=1)
    combine = jax.nn.one_hot(g_idx[:, 0], MOE_GROUPS, dtype=F32)[:, :, None] * g_val[:, :, None] * w_group[:, None, :]
    combine = combine.reshape(-1, N_EXPERTS).astype(t.dtype)
    gate = jnp.einsum('td,edf->tef', t, p['w_gate'])
    up = jnp.einsum('td,edf->tef', t, p['w_up'])
    hid = jax.nn.silu(gate) * up * combine[..., None]
    return jnp.einsum('tef,efd->td', hid, p['w_down']).reshape(b, L, d)


def setup_inputs(seed: int = 0) -> dict:
    key = jax.random.key(seed)
    ks = jax.random.split(key, 28)
    nrm = lambda k, shape, scale: jax.random.normal(k, shape, F32) * scale
    beta = (8.0 * DEPTH) ** -0.25
    dt0 = jnp.exp(jax.random.uniform(ks[9], (DEPTH, 2, SSD_HEADS), F32, math.log(1e-3), math.log(1e-1)))
    dt_bias = dt0 + jnp.log(-jnp.expm1(-dt0))
    a_log = jnp.log(jax.random.uniform(ks[10], (DEPTH, 2, SSD_HEADS), F32, 1.0, 16.0))
    return {
        'x': nrm(ks[0], (BATCH, SEQ, D_MODEL), 1.0),
        'c': nrm(ks[1], (BATCH, D_MODEL), 1.0),
        'ctx': nrm(ks[2], (BATCH, CTX_LEN, D_MODEL), 1.0),
        'c_ctx': nrm(ks[3], (D_MODEL,), 1.0),
        'w_mod': nrm(ks[4], (DEPTH, D_MODEL, 6 * D_MODEL), 0.5 * D_MODEL ** -0.5),
        'b_mod': nrm(ks[5], (DEPTH, 6 * D_MODEL), 0.02),
        'w_in': nrm(ks[6], (DEPTH, D_MODEL, D_IN_PROJ), D_MODEL ** -0.5),
        'conv_w': nrm(ks[7], (DEPTH, CONV_W, D_XBC), CONV_W ** -0.5),
        'conv_b': nrm(ks[8], (DEPTH, D_XBC), 0.02),
        'dt_bias': dt_bias,
        'a_log': a_log,
        'd_skip': 1.0 + nrm(ks[11], (DEPTH, SSD_HEADS), 0.05),
        'ssd_norm_g': 1.0 + nrm(ks[12], (DEPTH, D_SSD), 0.05),
        'q_norm_g': 1.0 + nrm(ks[13], (DEPTH, HEAD_DIM), 0.05),
        'k_norm_g': 1.0 + nrm(ks[14], (DEPTH, HEAD_DIM), 0.05),
        'w_out': nrm(ks[15], (DEPTH, D_MIX, D_MODEL), beta * D_MIX ** -0.5),
        'ln1_g': 1.0 + nrm(ks[16], (DEPTH, D_MODEL), 0.05),
        'ln1_b': nrm(ks[17], (DEPTH, D_MODEL), 0.02),
        'w_rg': nrm(ks[18], (DEPTH, D_MODEL, MOE_GROUPS), D_MODEL ** -0.5),
        'b_rg': nrm(ks[19], (DEPTH, MOE_GROUPS), 0.01),
        'w_re': nrm(ks[20], (DEPTH, D_MODEL, N_EXPERTS), D_MODEL ** -0.5),
        'b_re': nrm(ks[21], (DEPTH, N_EXPERTS), 0.01),
        'w_gate': nrm(ks[22], (DEPTH, N_EXPERTS, D_MODEL, D_EXPERT), D_MODEL ** -0.5),
        'w_up': nrm(ks[23], (DEPTH, N_EXPERTS, D_MODEL, D_EXPERT), D_MODEL ** -0.5),
        'w_down': nrm(ks[24], (DEPTH, N_EXPERTS, D_EXPERT, D_MODEL), beta * D_EXPERT ** -0.5),
        'ln2_g': 1.0 + nrm(ks[25], (DEPTH, D_MODEL), 0.05),
        'ln2_b': nrm(ks[26], (DEPTH, D_MODEL), 0.02),
    }


def reference(x, c, ctx, c_ctx, w_mod, b_mod, w_in, conv_w, conv_b, dt_bias, a_log, d_skip, ssd_norm_g,
              q_norm_g, k_norm_g, w_out, ln1_g, ln1_b, w_rg, b_rg, w_re, b_re, w_gate, w_up, w_down,
              ln2_g, ln2_b):
    n_rows = x.shape[1] // GRID_W
    cos, sin = axial_rope_tables(n_rows)
    alpha = (2.0 * DEPTH) ** 0.25
    silu_c = jax.nn.silu(c)
    silu_cc = jax.nn.silu(c_ctx)
    for l in range(DEPTH):
        last = l == DEPTH - 1
        p = {'w_in': w_in[l], 'conv_w': conv_w[l], 'conv_b': conv_b[l], 'dt_bias': dt_bias[l],
             'a_log': a_log[l], 'd_skip': d_skip[l], 'ssd_norm_g': ssd_norm_g[l], 'q_norm_g': q_norm_g[l],
             'k_norm_g': k_norm_g[l], 'w_out': w_out[l], 'w_rg': w_rg[l], 'b_rg': b_rg[l], 'w_re': w_re[l],
             'b_re': b_re[l], 'w_gate': w_gate[l], 'w_up': w_up[l], 'w_down': w_down[l]}
        sh_a, sc_a, g_a, sh_f, sc_f, g_f = [m[:, None, :] for m in jnp.split(silu_c @ w_mod[l] + b_mod[l], 6, axis=-1)]
        n_mod = 3 if last else 6
        mod_c = jnp.split(silu_cc @ w_mod[l][:, :n_mod * D_MODEL] + b_mod[l][:n_mod * D_MODEL], n_mod, axis=-1)
        hc = ctx * (1.0 + mod_c[1]) + mod_c[0]
        ctx_side, ctx_out = context_mixer(hc, p, not last)
        hx = x * (1.0 + sc_a) + sh_a
        x = layer_norm(alpha * x + g_a * latent_mixer(hx, ctx_side, p, cos, sin), ln1_g[l], ln1_b[l])
        x = layer_norm(alpha * x + g_f * hier_moe(x * (1.0 + sc_f) + sh_f, p), ln2_g[l], ln2_b[l])
        if not last:
            ctx = layer_norm(alpha * ctx + mod_c[2] * ctx_out, ln1_g[l], ln1_b[l])
            ctx = layer_norm(alpha * ctx + mod_c[5] * hier_moe(ctx * (1.0 + mod_c[4]) + mod_c[3], p),
                             ln2_g[l], ln2_b[l])
    return x
```

```python
import numpy as np
import concourse.bass as bass
import concourse.mybir as mybir
from concourse.bass_utils import run_bass_kernel_spmd

F32 = mybir.dt.float32
BF16 = mybir.dt.bfloat16
AF = mybir.ActivationFunctionType
ALU = mybir.AluOpType
AX = mybir.AxisListType

SB_BASE = 16512
SB_END = 229376


class Buf:
    def __init__(self, t, name):
        self.t = t
        self.name = name
        self.w = None
        self.r = {}
        self.dsem = None
        self.excl = False

    def __getitem__(self, k):
        return View(self, self.t[k])


class View:
    def __init__(self, buf, ap):
        self.buf = buf
        self.ap = ap

    def __getitem__(self, k):
        return View(self.buf, self.ap[k])

    def rearrange(self, s, **kw):
        return View(self.buf, self.ap.rearrange(s, **kw))

    def bcast(self, shape):
        return View(self.buf, self.ap.broadcast_to(list(shape)))

    def unsq(self, axis):
        return View(self.buf, self.ap.unsqueeze(axis))

    def bitcast(self, dt):
        return View(self.buf, self.ap.bitcast(dt))

    @property
    def shape(self):
        return tuple(self.ap.shape)


WRITE_KEYS = ("out", "accum_out", "ap")


class Prog:
    ENGS = ("pe", "act", "dve", "pool", "sp")

    def __init__(self, nc):
        self.nc = nc
        self.sems = {}
        self.cnt = {}
        self.seen = {e: {} for e in self.ENGS}
        self.streams = {e: [] for e in self.ENGS}
        self.top = SB_BASE
        self.ninst = 0
        for e in self.ENGS:
            self._mksem(e)

    def _mksem(self, name):
        self.sems[name] = self.nc.alloc_semaphore(name)
        self.cnt[name] = 0

    def sb(self, name, shape, dtype=F32):
        nbytes = int(np.prod(shape[1:])) * (4 if dtype == F32 else 2)
        nbytes = (nbytes + 63) // 64 * 64
        off = self.top
        assert off + nbytes <= SB_END, f"SBUF overflow allocating {name}: {off + nbytes - SB_BASE}"
        self.top += nbytes
        self.maxtop = max(getattr(self, 'maxtop', 0), self.top)
        uname = f"{name}_{off}"
        self.nalloc = getattr(self, "nalloc", 0) + 1
        return Buf(self.nc.alloc_sbuf_tensor_at(f"{uname}_{self.nalloc}", list(shape), dtype, offset=off), uname)

    def mark(self):
        return self.top

    def release(self, mark):
        self.barrier()
        self.top = mark

    def ps(self, name, shape, dtype=F32):
        b = Buf(self.nc.alloc_psum_tensor(name, list(shape), dtype), name)
        b.excl = True
        return b

    def dram(self, name, shape, dtype, kind="Internal"):
        return Buf(self.nc.dram_tensor(name, list(shape), dtype, kind=kind).ap(), name)

    @staticmethod
    def _add(deps, tok):
        if tok is None:
            return
        s, v = tok
        if deps.get(s, 0) < v:
            deps[s] = v

    def _deps(self, reads, writes):
        deps = {}
        for b in reads:
            self._add(deps, b.w)
        for b in writes:
            self._add(deps, b.w)
            for s, v in b.r.items():
                self._add(deps, (s, v))
        return deps

    def _emit_waits(self, e, deps, skip_self):
        seen = self.seen[e]
        for s, v in deps.items():
            if s == e and skip_self:
                continue
            if seen.get(s, 0) >= v:
                continue
            seen[s] = v
            sem = self.sems[s]
            self.streams[e].append(lambda eng, sem=sem, v=v: eng.wait_ge(sem, v))

    def _mark(self, tok, reads, writes):
        s, v = tok
        for b in reads:
            if b.r.get(s, 0) < v:
                b.r[s] = v
        for b in writes:
            b.w = tok
            b.r = {}

    @staticmethod
    def _split(kw):
        reads, writes, real = [], [], {}
        for k, a in kw.items():
            if isinstance(a, View):
                (writes if (k in WRITE_KEYS or a.buf.excl) else reads).append(a.buf)
                real[k] = a.ap
            else:
                real[k] = a
        return reads, writes, real

    def I(self, e, method, **kw):
        reads, writes, real = self._split(kw)
        if method == "matmul" and kw.get("start") is False:
            reads = reads + writes
        deps = self._deps(reads, writes)
        self._emit_waits(e, deps, skip_self=(e == "pe"))
        self.cnt[e] += 1
        sem = self.sems[e]
        self.streams[e].append(lambda eng, m=method, real=real, sem=sem: getattr(eng, m)(**real).then_inc(sem, 1))
        self._mark((e, self.cnt[e]), reads, writes)
        self.ninst += 1

    def dma(self, out, in_, q="sp", **kw):
        reads, writes = [], []
        if isinstance(in_, View):
            reads.append(in_.buf)
            in_ = in_.ap
        if isinstance(out, View):
            writes.append(out.buf)
            out = out.ap
        deps = self._deps(reads, writes)
        self._emit_waits(q, deps, skip_self=True)
        tb = writes[0] if writes else reads[0]
        if tb.dsem is None:
            tb.dsem = "d_" + tb.name
            if tb.dsem not in self.sems:
                self._mksem(tb.dsem)
        s = tb.dsem
        self.cnt[s] += 16
        sem = self.sems[s]
        self.streams[q].append(lambda eng, out=out, in_=in_, sem=sem, kw=kw: eng.dma_start(out=out, in_=in_, **kw).then_inc(sem, 16))
        self._mark((s, self.cnt[s]), reads, writes)
        self.ninst += 1

    def barrier(self):
        for e in self.ENGS:
            deps = {s: v for s, v in self.cnt.items() if v > 0}
            self._emit_waits(e, deps, skip_self=(e == "pe"))

    def build(self):
        with self.nc.Block() as block:
            def mk(e):
                def run(eng):
                    for f in self.streams[e]:
                        f(eng)
                return run
            block.tensor(mk("pe"))
            block.scalar(mk("act"))
            block.vector(mk("dve"))
            block.gpsimd(mk("pool"))
            block.sync(mk("sp"))


class Ring:
    def __init__(self, P, name, n, shape, dtype=F32):
        self.bufs = [P.sb(f"{name}{i}", shape, dtype) for i in range(n)]
        self.i = 0

    def get(self):
        b = self.bufs[self.i % len(self.bufs)]
        self.i += 1
        return b


OFF_Z, OFF_Q, OFF_XBC, OFF_DT, OFF_K, OFF_V, D_IN = 0, 1024, 2048, 3584, 3616, 3872, 4128
T = 2048
TC = 256
ALPHA = 2.0 ** 0.25
NB = 2


class StopBuild(Exception):
    pass


_last = [None, None]
used_inputs = []


def build_program(dbg=(), stop_after=None, nb=NB, skip=()):
    nc = bass.Bass("TRN2", target_bir_lowering=False)
    P = Prog(nc)
    _last[:] = [nc, P]

    in_shapes = {
        "x": [NB, T, 1024], "ctx": [NB, TC, 1024], "crow": [3, 1024], "w_mod": [1024, 6144], "b_mod": [1, 6144],
        "w_in": [1024, D_IN], "conv_w": [5, 1536], "conv_b": [1, 1536], "dt_bias": [1, 32], "a_log": [1, 32], "d_skip": [1, 16],
        "ssd_norm_g": [1, 1024], "q_norm_g": [64, 1], "k_norm_g": [64, 1], "w_out": [2048, 1024],
        "ln1_g": [1, 1024], "ln1_b": [1, 1024], "ln2_g": [1, 1024], "ln2_b": [1, 1024], "w_r": [1024, 20], "b_r": [1, 20],
        "w_gate": [16, 1024, 256], "w_up": [16, 1024, 256], "w_down": [16, 256, 1024],
        "c_ident": [128, 128], "c_tri": [5, 128, 128], "c_prot": [128, 128], "c_bones": [128, 128], "c_cos": [128, T], "c_sin": [128, T],
    }
    in_aps = {}
    used_inputs.clear()

    class _D:
        def __getattr__(self, name):
            if name not in in_aps:
                in_aps[name] = nc.dram_tensor(name, list(in_shapes[name]), F32, kind="ExternalInput").ap()
                used_inputs.append(name)
            return in_aps[name]
    D = _D()
    y_d = nc.dram_tensor("y", [NB, T, 1024], F32, kind="ExternalOutput").ap()
    cat = P.dram("cat", [16, 128, T], BF16)
    ybuf = Buf(y_d, "ybuf")

    def tap(name, view, shape, dt=F32):
        if name in dbg:
            o = nc.dram_tensor("dbg_" + name, list(shape), dt, kind="ExternalOutput").ap()
            P.dma(o, view)

    def stop(name):
        if stop_after == name:
            raise StopBuild()

    bk = [P.ps(f"bank{i}", [128, 512]) for i in range(4)]
    spair = []
    for i in range(2):
        pt_ = nc.alloc_psum_tensor(f"pp{i}", [128, 1024], F32)
        for vw in (pt_[:, 0:512], pt_[:, 512:1024]):
            b_ = Buf(vw, f"bank{len(bk)}"); b_.excl = True; bk.append(b_)
        b_ = Buf(pt_[:, :], f"spair{i}"); b_.excl = True; spair.append(b_)

    ident_f = P.sb("ident_f", [128, 128]); ident_b = P.sb("ident_b", [128, 128], BF16)
    tri = P.sb("tri", [128, 5, 128])
    TRI_LE, TRI_GE, TRI_GT, TRI_LT, TRI_ONE = range(5)
    prot_b = P.sb("prot_b", [128, 128], BF16); bones_b = P.sb("bones_b", [128, 128], BF16)
    convw = P.sb("convw", [128, 12, 5]); convb = P.sb("convb", [128, 12])
    gq = P.sb("gq", [128, 1]); gk = P.sb("gk", [128, 1])
    ssdg = P.sb("ssdg", [128, 1024]); dskip = P.sb("dskip", [128, 16]); dtb = P.sb("dtb", [128, 32]); Aneg = P.sb("Aneg", [128, 32])
    br = P.sb("br", [128, 20]); wr = P.sb("wr", [128, 8, 20])
    modT = P.sb("modT", [128, 48, 3])
    gA = P.sb("gA", [128, 1024]); gF = P.sb("gF", [128, 1024])
    eps5 = P.sb("eps5", [128, 1]); eps6 = P.sb("eps6", [128, 1])
    stage = Ring(P, "stage", 2, [128, 2048])
    xt = Ring(P, "xt", 4, [128, 1024])

    P.dma(ident_f[:], D.c_ident)
    P.dma(tri[:], D.c_tri.rearrange("k p q -> p k q"))
    P.I("pool", "tensor_copy", out=ident_b[:], in_=ident_f[:])
    s0 = stage.get()
    P.dma(s0[:, 0:128], D.c_prot); P.dma(s0[:, 128:256], D.c_bones)
    P.I("pool", "tensor_copy", out=prot_b[:], in_=s0[:, 0:128])
    P.I("pool", "tensor_copy", out=bones_b[:], in_=s0[:, 128:256])
    for c in range(12):
        P.dma(convw[:, c, :], D.conv_w[:, c * 128:(c + 1) * 128].rearrange("k p -> p k"), allow_slow_non_contiguous=True)
        P.dma(convb[:, c:c + 1], D.conv_b[:, c * 128:(c + 1) * 128].rearrange("k p -> p k"), allow_slow_non_contiguous=True)
    for h in range(2):
        P.dma(gq[h * 64:(h + 1) * 64, :], D.q_norm_g); P.dma(gk[h * 64:(h + 1) * 64, :], D.k_norm_g)
    P.dma(ssdg[:], D.ssd_norm_g.partition_broadcast(128)); P.dma(dskip[:], D.d_skip.partition_broadcast(128))
    P.dma(dtb[:], D.dt_bias.partition_broadcast(128)); P.dma(Aneg[:], D.a_log.partition_broadcast(128))
    P.dma(br[:], D.b_r.partition_broadcast(128))
    P.dma(wr[:], D.w_r.rearrange("(k p) n -> p k n", p=128))
    P.I("act", "activation", out=Aneg[:], in_=Aneg[:], func=AF.Exp)
    P.I("dve", "tensor_scalar", out=Aneg[:], in0=Aneg[:], scalar1=-1.0, scalar2=None, op0=ALU.mult)
    P.I("dve", "memset", ap=eps5[:], constant=1e-5); P.I("dve", "memset", ap=eps6[:], constant=1e-6)

    m0 = P.mark()
    crow = P.sb("crow", [3, 1024]); scT = P.sb("scT", [128, 8, 3]); bmod3 = P.sb("bmod3", [3, 6144]); modrow = P.sb("modrow", [3, 6144])
    P.dma(crow[:], D.crow)
    P.dma(bmod3[:], D.b_mod.partition_broadcast(3))
    P.I("act", "activation", out=crow[:], in_=crow[:], func=AF.Silu)
    for kc in range(8):
        P.I("pe", "transpose", out=bk[0][:, kc * 3:(kc + 1) * 3], in_=crow[0:3, kc * 128:(kc + 1) * 128], identity=ident_f[0:3, 0:3])
    P.I("dve", "tensor_copy", out=scT[:].rearrange("p k r -> p (k r)"), in_=bk[0][:, 0:24])
    wm_r = Ring(P, "wmst", 6, [128, 2048])
    for third in range(3):
        for kc in range(8):
            st = wm_r.get()
            P.dma(st[:], D.w_mod[kc * 128:(kc + 1) * 128, third * 2048:(third + 1) * 2048])
            for j in range(4):
                P.I("pe", "matmul", out=bk[1 + j][0:3, :], lhsT=scT[:, kc, :], rhs=st[:, j * 512:(j + 1) * 512], start=(kc == 0), stop=(kc == 7))
        for j in range(4):
            c0 = third * 2048 + j * 512
            P.I("dve", "tensor_tensor", out=modrow[:, c0:c0 + 512], in0=bk[1 + j][0:3, :], in1=bmod3[:, c0:c0 + 512], op=ALU.add)
    for j in range(48):
        P.I("pe", "transpose", out=bk[0][:, j * 3:(j + 1) * 3], in_=modrow[0:3, j * 128:(j + 1) * 128], identity=ident_f[0:3, 0:3])
    P.I("dve", "tensor_copy", out=modT[:].rearrange("p k r -> p (k r)"), in_=bk[0][:, 0:144])
    P.I("dve", "tensor_scalar", out=modT[:, 8:16, :], in0=modT[:, 8:16, :], scalar1=1.0, scalar2=None, op0=ALU.add)
    P.I("dve", "tensor_scalar", out=modT[:, 32:40, :], in0=modT[:, 32:40, :], scalar1=1.0, scalar2=1.0 / ALPHA, op0=ALU.add, op1=ALU.mult)
    tap("modT", modT[:].rearrange("p k r -> p (k r)"), [128, 144])
    P.release(m0)
    stop("mod")

    def bcast_row(dst, sec, row):
        rep = stage.get()
        for c in range(8):
            P.I("dve", "tensor_copy", out=rep[:, c * 128:(c + 1) * 128], in_=modT[:, sec * 8 + c, row:row + 1].bcast([128, 128]))
        for c in range(8):
            P.I("pe", "matmul", out=bk[c // 4][:, (c % 4) * 128:(c % 4 + 1) * 128], lhsT=rep[:, c * 128:(c + 1) * 128], rhs=ident_f[:], start=True, stop=True)
        for hh in range(2):
            P.I("act", "activation", out=dst[:, hh * 512:(hh + 1) * 512], in_=bk[hh][:], func=AF.Identity)

    for b in range(nb):
        mb = P.mark()
        bcast_row(gA, 2, b)
        bcast_row(gF, 5, b)
        tap(f"gA{b}", gA[:], [128, 1024])
        hxT = P.sb("hxT", [128, 8, T + 4], BF16)
        hcT = P.sb("hcT", [128, 8, TC + 4], BF16)
        P.I("pool", "memset", ap=hxT[:, :, 0:2], constant=0.0); P.I("pool", "memset", ap=hxT[:, :, T + 2:T + 4], constant=0.0)
        P.I("pool", "memset", ap=hcT[:, :, 0:2], constant=0.0); P.I("pool", "memset", ap=hcT[:, :, TC + 2:TC + 4], constant=0.0)

        def build_hT(dstT, src_d, ntile, row):
            for blk in range((ntile + 3) // 4):
                nt = min(4, ntile - blk * 4)
                tiles = []
                for tt in range(nt):
                    t_ = xt.get()
                    P.dma(t_[:], src_d[(blk * 4 + tt) * 128:(blk * 4 + tt + 1) * 128, :])
                    tiles.append(t_)
                for c in range(8):
                    bank = bk[c % 4]
                    for tt in range(nt):
                        P.I("pe", "transpose", out=bank[:, tt * 128:(tt + 1) * 128], in_=tiles[tt][:, c * 128:(c + 1) * 128], identity=ident_f[:])
                    dst = dstT[:, c, 2 + blk * 512:2 + blk * 512 + nt * 128]
                    if c % 2 == 0:
                        P.I("act", "activation", out=dst, in_=bank[:, 0:nt * 128], func=AF.Identity, scale=modT[:, 8 + c, row:row + 1], bias=modT[:, c, row:row + 1])
                    else:
                        P.I("dve", "tensor_scalar", out=dst, in0=bank[:, 0:nt * 128], scalar1=modT[:, 8 + c, row:row + 1], scalar2=modT[:, c, row:row + 1], op0=ALU.mult, op1=ALU.add)

        build_hT(hcT, D.ctx[b], 2, 2)
        build_hT(hxT, D.x[b], 16, b)
        if b == 0:
            tap("hxT", hxT[:].rearrange("p k t -> p (k t)"), [128, 8 * (T + 4)], BF16)
            tap("hcT", hcT[:].rearrange("p k t -> p (k t)"), [128, 8 * (TC + 4)], BF16)
        stop("hx")

        def load_cols(dst_w, col_specs):
            for kc in range(8):
                st = stage.get()
                o = 0
                for (c0, n) in col_specs:
                    P.dma(st[:, o:o + n], D.w_in[kc * 128:(kc + 1) * 128, c0:c0 + n])
                    o += n
                P.I("pool", "tensor_copy", out=dst_w[:, kc, 0:o], in_=st[:, 0:o])

        hv = lambda v: v.rearrange("p (h j) -> p h j", h=8)

        def ssd_pass(g):
            mp = P.mark()
            wZ = P.sb("wZ", [128, 8, 512], BF16)
            load_cols(wZ, [(OFF_Z + g * 512, 512)])
            cidx = [g * 4, g * 4 + 1, g * 4 + 2, g * 4 + 3, 8 + g, 10 + g]

            dtA = P.sb("dtA", [128, 18, 16]); aA = P.sb("aA", [128, 18, 16])
            ewA = [P.sb("ewF", [128, 18, 16]), P.sb("ewB", [128, 18, 16])]

            def mkstore(tag, n, i0):
                return [dict(xs=P.sb(f"xs{tag}{i}", [128, 512], BF16), Bt=P.sb(f"Bt{tag}{i}", [128, 128], BF16),
                             BT=P.sb(f"BT{tag}{i}", [128, 128], BF16), CT=P.sb(f"CT{tag}{i}", [128, 128], BF16),
                             dt=dtA[:, i0 + i, :], a=aA[:, i0 + i, :], idx=i0 + i) for i in range(n)]
            lat = mkstore("L", 16, 0)
            hin_b = [P.sb(f"hinb{i}", [128, 512], BF16) for i in range(16)]
            hin_f = Ring(P, "hinf", 3, [128, 512], BF16)
            h_f = P.sb("h_f", [128, 512]); h_b = P.sb("h_b", [128, 512])
            sm_r = {n: Ring(P, "sm_" + n, 2, [128, 16]) for n in ("sc", "ecol")}
            xw_r = Ring(P, "xw", 2, [128, 512], BF16)

            def update(h, st, dirn, bank_s):
                sc = sm_r["sc"].get(); xw = xw_r.get()
                ew = ewA[dirn][:, st["idx"], :]
                P.I("dve", "tensor_tensor", out=sc[:, 0:8], in0=st["dt"][:, dirn * 8:(dirn + 1) * 8], in1=ew[:, 0:8], op=ALU.mult)
                P.I("dve", "tensor_tensor", out=hv(xw[:]), in0=hv(st["xs"][:]), in1=sc[:, 0:8].unsq(2).bcast([128, 8, 64]), op=ALU.mult)
                P.I("pe", "matmul", out=bank_s[:], lhsT=st["Bt"][:], rhs=xw[:], start=True, stop=True)
                P.I("pool", "tensor_tensor", out=hv(h[:]), in0=hv(h[:]), in1=ew[:, 8:16].unsq(2).bcast([128, 8, 64]), op=ALU.mult)
                P.I("dve", "tensor_tensor", out=h[:], in0=bank_s[:], in1=h[:], op=ALU.add)

            m1 = P.mark()
            wS = P.sb("wS", [128, 8, 784], BF16)
            load_cols(wS, [(OFF_XBC + g * 512, 512), (OFF_XBC + 1024 + g * 128, 128), (OFF_XBC + 1280 + g * 128, 128),
                           (OFF_DT + g * 8, 8), (OFF_DT + 16 + g * 8, 8)])
            cst = mkstore("C", 2, 16)
            rawS_r = Ring(P, "rawS", 2, [128, 6, 132], BF16); xcT_r = Ring(P, "xcT", 2, [128, 4, 128], BF16)
            dg = P.sb("dg", [128, 6, 5, 128], BF16)
            for r in range(6):
                for k in range(5):
                    P.I("dve", "tensor_scalar", out=dg[:, r, k, :], in0=ident_f[:], scalar1=convw[:, cidx[r], k:k + 1], scalar2=None, op0=ALU.mult)

            def prep(hT, c, st, need_C):
                rawS = rawS_r.get(); xcT = xcT_r.get()
                tok0 = c * 128
                nr = 6 if need_C else 5
                for r in range(nr):
                    bank = bk[r // 3]
                    o = (r % 3) * 132
                    for kc in range(8):
                        P.I("pe", "matmul", out=bank[:, o:o + 132], lhsT=wS[:, kc, r * 128:(r + 1) * 128], rhs=hT[:, kc, tok0:tok0 + 132], start=(kc == 0), stop=(kc == 7))
                P.I("act", "activation", out=rawS[:, 0:3, :].rearrange("p r t -> p (r t)"), in_=bk[0][:, 0:396], func=AF.Identity)
                P.I("act", "activation", out=rawS[:, 3:nr, :].rearrange("p r t -> p (r t)"), in_=bk[1][:, 0:(nr - 3) * 132], func=AF.Identity)
                for r in range(nr):
                    cb = bk[2][:, r * 128:(r + 1) * 128] if r < 4 else bk[3][:, (r - 4) * 128:(r - 3) * 128]
                    for k in range(5):
                        P.I("pe", "matmul", out=cb, lhsT=dg[:, r, k, :], rhs=rawS[:, r, k:k + 128], start=(k == 0), stop=(k == 4))
                for r in range(nr):
                    cb = bk[2][:, r * 128:(r + 1) * 128] if r < 4 else bk[3][:, (r - 4) * 128:(r - 3) * 128]
                    dst = xcT[:, r, :] if r < 4 else (st["BT"][:] if r == 4 else st["CT"][:])
                    P.I("act", "activation", out=dst, in_=cb, func=AF.Silu, bias=convb[:, cidx[r]:cidx[r] + 1])
                for r in range(4):
                    P.I("pe", "matmul", out=bk[6][:, r * 128:(r + 1) * 128], lhsT=xcT[:, r, :], rhs=ident_b[:], start=True, stop=True)
                P.I("pe", "matmul", out=bk[7][:, 0:128], lhsT=st["BT"][:], rhs=ident_b[:], start=True, stop=True)
                P.I("act", "activation", out=st["xs"][:], in_=bk[6][:], func=AF.Identity)
                P.I("dve", "tensor_copy", out=st["Bt"][:], in_=bk[7][:, 0:128])

            big = {n: P.sb("big_" + n, [128, 18, 16]) for n in ("u", "nu", "na", "e", "l")}
            for idx in range(18):
                hT_, tok0_ = (hxT, idx * 128) if idx < 16 else (hcT, (idx - 16) * 128)
                for kc in range(8):
                    P.I("pe", "matmul", out=bk[3][:, idx * 16:(idx + 1) * 16], lhsT=hT_[:, kc, 2 + tok0_:2 + tok0_ + 128], rhs=wS[:, kc, 768:784], start=(kc == 0), stop=(kc == 7))
            d4 = lambda v: v.rearrange("p i (d h) -> p i d h", d=2)
            fl = lambda v: v.rearrange("p i x -> p (i x)")
            dvg = lambda v: v.rearrange("p (d h) -> p d h", d=2)[:, :, g * 8:(g + 1) * 8].unsq(1).bcast([128, 18, 2, 8])
            P.I("dve", "tensor_tensor", out=d4(big["u"][:]), in0=d4(bk[3][:, 0:288].rearrange("p (i x) -> p i x", i=18)), in1=dvg(dtb[:]), op=ALU.add)
            P.I("dve", "tensor_scalar", out=fl(big["nu"][:]), in0=fl(big["u"][:]), scalar1=-1.0, scalar2=None, op0=ALU.mult)
            P.I("dve", "tensor_tensor", out=fl(big["na"][:]), in0=fl(big["u"][:]), in1=fl(big["nu"][:]), op=ALU.min)
            P.I("act", "activation", out=fl(big["e"][:]), in_=fl(big["na"][:]), func=AF.Exp)
            P.I("act", "activation", out=fl(big["l"][:]), in_=fl(big["e"][:]), func=AF.Ln, bias=1.0, scale=1.0)
            P.I("dve", "scalar_tensor_tensor", out=fl(dtA[:]), in0=fl(big["u"][:]), scalar=0.0, in1=fl(big["l"][:]), op0=ALU.max, op1=ALU.add)
            P.I("dve", "tensor_tensor", out=d4(aA[:]), in0=d4(dtA[:]), in1=dvg(Aneg[:]), op=ALU.mult)
            for dirn in range(2):
                for idx in range(18):
                    a_d = aA[:, idx, dirn * 8:(dirn + 1) * 8]
                    P.I("pe", "matmul", out=bk[4 + dirn][:, idx * 16:idx * 16 + 8], lhsT=tri[:, TRI_GT if dirn == 0 else TRI_LT, :], rhs=a_d, start=True, stop=True)
                    P.I("pe", "matmul", out=bk[4 + dirn][:, idx * 16 + 8:idx * 16 + 16], lhsT=tri[:, TRI_ONE, :], rhs=a_d, start=True, stop=True)
                P.I("act", "activation", out=fl(ewA[dirn][:]), in_=bk[4 + dirn][:, 0:288], func=AF.Exp)

            for cc in range(2):
                prep(hcT, cc, cst[cc], False)
            P.I("pool", "memset", ap=h_f[:], constant=0.0); P.I("pool", "memset", ap=h_b[:], constant=0.0)
            prep(hxT, 15, lat[15], True)
            update(h_f, cst[0], 0, bk[5]); update(h_f, cst[1], 0, bk[5])
            update(h_b, cst[1], 1, bk[5]); update(h_b, cst[0], 1, bk[5])
            if b == 0 and g == 0:
                tap("h0f", h_f[:], [128, 512]); tap("h0b", h_b[:], [128, 512])
            for c in range(15, -1, -1):
                if c > 0:
                    prep(hxT, c - 1, lat[c - 1], True)
                P.I("pool", "tensor_copy", out=hin_b[c][:], in_=h_b[:])
                update(h_b, lat[c], 1, bk[4 + c % 2])
            stop("ssd_s1")
            P.release(m1)

            zs_r = Ring(P, "zs", 2, [128, 512]); CBmF_r = Ring(P, "CBmF", 2, [128, 128]); CBmB_r = Ring(P, "CBmB", 2, [128, 128])
            yoffF_r = Ring(P, "yoffF", 2, [128, 512], BF16); yoffB_r = Ring(P, "yoffB", 2, [128, 512], BF16); xskip_r = Ring(P, "xskip", 2, [128, 512], BF16)
            arepm_r = Ring(P, "arepm", 2, [128, 8, 128]); xdt_r = Ring(P, "xdt", 4, [128, 512], BF16); Lt_r = Ring(P, "Lt", 2, [128, 512])
            Mt_r = Ring(P, "Mt", 8, [128, 4, 128], BF16)
            yz_r = Ring(P, "yz", 2, [128, 512]); junk = P.sb("junk", [128, 512], BF16)
            ssq_r = Ring(P, "ssq", 2, [128, 1]); sd_r = Ring(P, "sd", 2, [128, 1]); rstd_r = Ring(P, "rstd", 2, [128, 1])
            yn_r = Ring(P, "yn", 2, [128, 512], BF16); ynT = P.sb("ynT", [128, 4, 512], BF16)

            def mainA(c):
                st = lat[c]
                A = dict(zs=zs_r.get(), CBm=[CBmF_r.get(), CBmB_r.get()], ecol=sm_r["ecol"].get(), xskip=xskip_r.get(), xdt=[], Mt=[])
                for kc in range(8):
                    P.I("pe", "matmul", out=bk[0][:], lhsT=hxT[:, kc, 2 + c * 128:2 + (c + 1) * 128], rhs=wZ[:, kc, :], start=(kc == 0), stop=(kc == 7))
                P.I("act", "activation", out=A["zs"][:], in_=bk[0][:], func=AF.Silu)
                P.I("pe", "matmul", out=bk[1][:, 0:128], lhsT=st["BT"][:], rhs=st["CT"][:], start=True, stop=True)
                P.I("pe", "matmul", out=bk[1][:, 128:136], lhsT=tri[:, TRI_LE, :], rhs=st["a"][:, 0:8], start=True, stop=True)
                P.I("pe", "matmul", out=bk[1][:, 136:144], lhsT=tri[:, TRI_GE, :], rhs=st["a"][:, 8:16], start=True, stop=True)
                P.I("dve", "tensor_tensor", out=A["CBm"][0][:], in0=bk[1][:, 0:128], in1=tri[:, TRI_LE, :], op=ALU.mult)
                P.I("dve", "tensor_tensor", out=A["CBm"][1][:], in0=bk[1][:, 0:128], in1=tri[:, TRI_GE, :], op=ALU.mult)
                P.I("act", "activation", out=A["ecol"][:], in_=bk[1][:, 128:144], func=AF.Exp)
                P.I("pool", "tensor_tensor", out=hv(A["xskip"][:]), in0=hv(st["xs"][:]), in1=dskip[:, g * 8:(g + 1) * 8].unsq(2).bcast([128, 8, 64]), op=ALU.mult)
                for dirn in range(2):
                    a_d = st["a"][:, dirn * 8:(dirn + 1) * 8]
                    arepm = arepm_r.get(); xdt = xdt_r.get()
                    A["xdt"].append(xdt)
                    P.I("dve", "tensor_tensor", out=arepm[:], in0=a_d.unsq(2).bcast([128, 8, 128]),
                        in1=tri[:, TRI_GT if dirn == 0 else TRI_LT, :].unsq(1).bcast([128, 8, 128]), op=ALU.mult)
                    P.I("pool", "tensor_tensor", out=hv(xdt[:]), in0=hv(st["xs"][:]), in1=st["dt"][:, dirn * 8:(dirn + 1) * 8].unsq(2).bcast([128, 8, 64]), op=ALU.mult)
                    for hb in range(2):
                        bankD = bk[5 + hb]
                        Lt = Lt_r.get(); Mt = Mt_r.get()
                        A["Mt"].append(Mt)
                        for h in range(4):
                            P.I("pe", "matmul", out=bankD[:, h * 128:(h + 1) * 128], lhsT=arepm[:, hb * 4 + h, :], rhs=tri[:, TRI_LE if dirn == 0 else TRI_GE, :], start=True, stop=True)
                        P.I("act", "activation", out=Lt[:], in_=bankD[:], func=AF.Exp)
                        P.I("dve" if (dirn == 1 and hb == 1) else "pool", "tensor_tensor", out=Mt[:], in0=Lt[:].rearrange("p (h q) -> p h q", h=4), in1=A["CBm"][dirn][:].unsq(1).bcast([128, 4, 128]), op=ALU.mult)
                return A

            def mainB(c, A, hinf):
                st = lat[c]
                yoff = [yoffF_r.get(), yoffB_r.get()]; yz = yz_r.get(); ssq = ssq_r.get(); sd = sd_r.get(); rstd = rstd_r.get(); yn = yn_r.get()
                P.I("pe", "matmul", out=bk[2][:], lhsT=st["CT"][:], rhs=hinf[:], start=True, stop=True)
                P.I("pe", "matmul", out=bk[3][:], lhsT=st["CT"][:], rhs=hin_b[c][:], start=True, stop=True)
                for dirn in range(2):
                    P.I("dve", "tensor_tensor", out=hv(yoff[dirn][:]), in0=hv(bk[2 + dirn][:]), in1=A["ecol"][:, dirn * 8:(dirn + 1) * 8].unsq(2).bcast([128, 8, 64]), op=ALU.mult)
                P.I("pe", "matmul", out=bk[4][:], lhsT=ident_b[:], rhs=A["xskip"][:], start=True, stop=False)
                P.I("pe", "matmul", out=bk[4][:], lhsT=ident_b[:], rhs=yoff[0][:], start=False, stop=False)
                P.I("pe", "matmul", out=bk[4][:], lhsT=ident_b[:], rhs=yoff[1][:], start=False, stop=False)
                for dirn in range(2):
                    for hb in range(2):
                        Mt = A["Mt"][dirn * 2 + hb]; xdt = A["xdt"][dirn]
                        for h in range(4):
                            hh = hb * 4 + h
                            P.I("pe", "matmul", out=bk[4][:, hh * 64:(hh + 1) * 64], lhsT=Mt[:, h, :], rhs=xdt[:, hh * 64:(hh + 1) * 64], start=False, stop=(dirn == 1 and hh == 7))
                P.I("dve", "tensor_tensor", out=yz[:], in0=bk[4][:], in1=A["zs"][:], op=ALU.mult)
                P.I("act", "activation", out=junk[:], in_=yz[:], func=AF.Square, accum_out=ssq[:])
                P.I("act", "activation", out=sd[:], in_=ssq[:], func=AF.Sqrt, scale=1.0 / 512.0, bias=eps6[:, 0:1])
                P.I("dve", "reciprocal", out=rstd[:], in_=sd[:])
                P.I("dve", "scalar_tensor_tensor", out=yn[:], in0=yz[:], scalar=rstd[:, 0:1], in1=ssdg[:, g * 512:(g + 1) * 512], op0=ALU.mult, op1=ALU.mult)
                for fc in range(4):
                    P.I("pe", "matmul", out=bk[7][:, fc * 128:(fc + 1) * 128], lhsT=yn[:, fc * 128:(fc + 1) * 128], rhs=ident_b[:], start=True, stop=True)
                P.I("act", "activation", out=ynT[:, :, (c % 4) * 128:(c % 4 + 1) * 128], in_=bk[7][:].rearrange("p (f t) -> p f t", f=4), func=AF.Identity)
                if c % 4 == 3:
                    for fc in range(4):
                        P.dma(cat[g * 4 + fc, :, (c // 4) * 512:(c // 4 + 1) * 512], ynT[:, fc, :])

            if b == 0 and g == 0:
                tap("xs0", lat[0]["xs"][:], [128, 512], BF16)
            A_next = mainA(0)
            for c in range(16):
                hf16 = hin_f.get()
                P.I("act", "activation", out=hf16[:], in_=h_f[:], func=AF.Identity)
                update(h_f, lat[c], 0, bk[7])
                A_cur = A_next
                if c + 1 < 16:
                    A_next = mainA(c + 1)
                mainB(c, A_cur, hf16)
            P.release(mp)

        for g in range(0 if "ssd" not in skip else 2, 2):
            ssd_pass(g)
            if b == 0 and g == 0:
                stop("ssd0")
        stop("ssd")

        ma = P.mark()
        cosT = P.sb("cosT", [128, T]); sinT = P.sb("sinT", [128, T])
        P.dma(cosT[:], D.c_cos); P.dma(sinT[:], D.c_sin)
        wsets = [dict(wq=P.sb(f"wq{i}", [128, 8, 256], BF16), wkd=P.sb(f"wkd{i}", [128, 8, 128], BF16), wv=P.sb(f"wv{i}", [128, 8, 64], BF16)) for i in range(2)]
        asets = [dict(KT=P.sb(f"KT{i}", [128, TC + T], BF16), VA=P.sb(f"VA{i}", [128, 18, 128], BF16), VB=P.sb(f"VB{i}", [128, 18, 128], BF16),
                      qT=P.sb(f"qT{i}", [128, 2, T], BF16)) for i in range(2)]
        for a_ in asets:
            P.I("pool", "memset", ap=a_["VA"][:], constant=1.0); P.I("pool", "memset", ap=a_["VB"][:], constant=1.0)
        nr_r = {n: Ring(P, n, 2, [128, 512], dt_) for n, dt_ in (("kgb", BF16), ("sqb", BF16), ("sdn", F32), ("rsn", F32), ("t1", F32), ("t2", F32))}
        PT = Ring(P, "PT", 3, [128, 1024], BF16); oTr = Ring(P, "oT", 2, [128, 2, 512], BF16); rec = P.sb("rec", [128, 512])

        def load_w(g):
            W = wsets[g % 2]
            for kc in range(8):
                st = stage.get()
                rows = slice(kc * 128, (kc + 1) * 128)
                P.dma(st[:, 0:256], D.w_in[rows, OFF_Q + g * 256:OFF_Q + (g + 1) * 256])
                P.dma(st[:, 256:320], D.w_in[rows, OFF_K + g * 64:OFF_K + (g + 1) * 64])
                P.dma(st[:, 320:384], D.w_in[rows, OFF_V + g * 64:OFF_V + (g + 1) * 64])
                P.I("pool", "tensor_copy", out=W["wq"][:, kc, :], in_=st[:, 0:256])
                P.I("pool", "tensor_copy", out=W["wkd"][:, kc, 0:64], in_=st[:, 256:320])
                P.I("pool", "tensor_copy", out=W["wkd"][:, kc, 64:128], in_=st[:, 256:320])
                P.I("pool", "tensor_copy", out=W["wv"][:, kc, :], in_=st[:, 320:384])

        def norm_rope(gvec, dst, n, tok0, bp, bs):
            kgb, sqb, sdn, rsn, t1, t2 = (nr_r[n_].get() for n_ in ("kgb", "sqb", "sdn", "rsn", "t1", "t2"))
            yield
            P.I("act", "activation", out=kgb[:, 0:n], in_=bp[:, 0:n], func=AF.Identity, scale=gvec[:, 0:1])
            P.I("act", "activation", out=sqb[:, 0:n], in_=bp[:, 0:n], func=AF.Square)
            yield
            P.I("pe", "matmul", out=bs[:, 0:n], lhsT=bones_b[:], rhs=sqb[:, 0:n], start=True, stop=True)
            if tok0 is not None:
                P.I("pe", "matmul", out=bp[:, 0:n], lhsT=prot_b[:], rhs=kgb[:, 0:n], start=True, stop=True)
                P.I("pool", "tensor_tensor", out=t1[:, 0:n], in0=kgb[:, 0:n], in1=cosT[:, tok0:tok0 + n], op=ALU.mult)
            yield
            P.I("act", "activation", out=sdn[:, 0:n], in_=bs[:, 0:n], func=AF.Sqrt, scale=1.0 / 64.0, bias=eps6[:, 0:1])
            if tok0 is not None:
                P.I("dve", "tensor_tensor", out=t2[:, 0:n], in0=bp[:, 0:n], in1=sinT[:, tok0:tok0 + n], op=ALU.mult)
            yield
            P.I("dve", "reciprocal", out=rsn[:, 0:n], in_=sdn[:, 0:n])
            if tok0 is None:
                yield
                P.I("dve", "tensor_tensor", out=dst, in0=kgb[:, 0:n], in1=rsn[:, 0:n], op=ALU.mult)
            else:
                P.I("pool", "tensor_tensor", out=t1[:, 0:n], in0=t1[:, 0:n], in1=t2[:, 0:n], op=ALU.add)
                yield
                P.I("dve", "tensor_tensor", out=dst, in0=t1[:, 0:n], in1=rsn[:, 0:n], op=ALU.mult)
            yield

        def qkv_units(g):
            W = wsets[g % 2]; S_ = asets[g % 2]
            ucnt = [0]

            def banks():
                ucnt[0] += 1
                return (bk[0], bk[1]) if ucnt[0] % 2 else (bk[2], bk[3])

            def vunit(hT, col0, t0, nt):
                _, bs = banks()
                for tt in range(nt):
                    for kc in range(8):
                        P.I("pe", "matmul", out=bs[:, tt * 64:(tt + 1) * 64], lhsT=hT[:, kc, col0 + tt * 128:col0 + (tt + 1) * 128], rhs=W["wv"][:, kc, :], start=(kc == 0), stop=(kc == 7))
                src = bs[:, 0:nt * 64].rearrange("p (t d) -> p t d", t=nt)
                yield
                P.I("act", "activation", out=S_["VA"][:, t0:t0 + nt, 0:64], in_=src, func=AF.Identity)
                P.I("dve", "tensor_copy", out=S_["VB"][:, t0:t0 + nt, 64:128], in_=src)

            bp, bs = banks()
            for kc in range(8):
                P.I("pe", "matmul", out=bp[:, 0:TC], lhsT=W["wkd"][:, kc, :], rhs=hcT[:, kc, 2:2 + TC], start=(kc == 0), stop=(kc == 7))
            yield from norm_rope(gk, S_["KT"][:, 0:TC], TC, None, bp, bs)
            yield from vunit(hcT, 2, 0, 2)
            yield
            for blk in range(4):
                tok0 = blk * 512
                bp, bs = banks()
                for kc in range(8):
                    P.I("pe", "matmul", out=bp[:], lhsT=W["wkd"][:, kc, :], rhs=hxT[:, kc, 2 + tok0:2 + tok0 + 512], start=(kc == 0), stop=(kc == 7))
                yield from norm_rope(gk, S_["KT"][:, TC + tok0:TC + tok0 + 512], 512, tok0, bp, bs)
                yield from vunit(hxT, 2 + tok0, 2 + blk * 4, 4)
                yield
                for j in range(2):
                    bp, bs = banks()
                    for kc in range(8):
                        P.I("pe", "matmul", out=bp[:], lhsT=W["wq"][:, kc, j * 128:(j + 1) * 128], rhs=hxT[:, kc, 2 + tok0:2 + tok0 + 512], start=(kc == 0), stop=(kc == 7))
                    yield from norm_rope(gq, S_["qT"][:, j, tok0:tok0 + 512], 512, tok0, bp, bs)

        sbanks = [bk[4], bk[5], bk[6], bk[7]]; obanks4 = [bk[0], bk[1], bk[2], bk[3]]
        its = [(qb, j, kt) for qb in range(4) for j in range(2) for kt in range(18)]

        def main_loop(g, filler):
            S_ = asets[g % 2]
            KT, VA, VB, qT = S_["KT"], S_["VA"], S_["VB"], S_["qT"]

            def s_mm(i):
                qb, j, kt = its[i]
                for half in range(2):
                    rows = slice(half * 64, half * 64 + 64)
                    P.I("pe", "matmul", out=spair[i % 2][:, half * 512:(half + 1) * 512], lhsT=KT[rows, kt * 128:(kt + 1) * 128], rhs=qT[rows, j, qb * 512:(qb + 1) * 512], start=True, stop=True)

            s_mm(0)
            oTb = None
            for i, (qb, j, kt) in enumerate(its):
                if i + 1 < len(its):
                    s_mm(i + 1)
                if j == 0 and kt == 0:
                    oTb = oTr.get()
                ptp = PT.get()
                obanks = obanks4[((i // 18) % 2) * 2:((i // 18) % 2) * 2 + 2]
                P.I("act", "activation", out=ptp[:], in_=spair[i % 2][:], func=AF.Exp, scale=0.125)
                for half in range(2):
                    P.I("pe", "matmul", out=obanks[half][:], lhsT=(VA if half == 0 else VB)[:, kt, :], rhs=ptp[:, half * 512:(half + 1) * 512], start=(kt == 0), stop=(kt == 17))
                if kt == 17:
                    P.I("dve", "reciprocal", out=rec[0:64, :], in_=obanks[0][64:128, :])
                    P.I("dve", "tensor_tensor", out=oTb[0:64, j, :], in0=obanks[0][0:64, :], in1=rec[0:64, :], op=ALU.mult)
                    P.I("dve", "reciprocal", out=rec[64:128, :], in_=obanks[1][0:64, :])
                    P.I("dve", "tensor_tensor", out=oTb[64:128, j, :], in0=obanks[1][64:128, :], in1=rec[64:128, :], op=ALU.mult)
                    if j == 1:
                        for jj in range(2):
                            P.dma(cat[8 + g * 2 + jj, :, qb * 512:(qb + 1) * 512], oTb[:, jj, :])
                if filler is not None:
                    next(filler, None)
            if filler is not None:
                for _ in filler:
                    pass

        load_w(0)
        for _ in qkv_units(0):
            pass
        load_w(1)
        for g in range(4):
            if g + 2 < 4:
                load_w(g + 2)
            main_loop(g, None)
            if g + 1 < 4:
                for _ in qkv_units(g + 1):
                    pass
        P.release(ma)
        stop("attn")
        P.release(mb)

        acc = [P.sb(f"acc{i}", [128, 1024]) for i in range(16)]
        hT = [P.sb(f"hT{i}", [128, 8, 512], BF16) for i in range(4)]
        comb = P.sb("comb", [128, 16, 16])
        sml = {n: P.sb("r_" + n, [128, w]) for n, w in (("lgt", 20), ("m", 1), ("negm", 1), ("oh", 4), ("eg", 4), ("se", 1), ("gval", 1), ("prod", 16),
                                                          ("esel", 4), ("m1", 1), ("oh1", 4), ("msk", 4), ("m2", 1), ("oh2", 4), ("d12", 1), ("e21", 1),
                                                          ("den", 1), ("w1", 1), ("w2", 1), ("tq", 4), ("wg", 4), ("ohg", 4), ("s1", 1), ("nm", 1),
                                                          ("ssq", 1), ("sd", 1), ("rstd", 1))}
        junkf = P.sb("junkf", [128, 1024], BF16)
        mo2 = P.mark()
        woH = P.sb("woH", [128, 8, 1024], BF16); catb_r = Ring(P, "catb", 2, [128, 8, 512], BF16)
        lng = P.sb("lng", [128, 1024]); lnb = P.sb("lnb", [128, 1024]); hf = P.sb("hf", [128, 8, 128])
        P.dma(lng[:], D.ln1_g.partition_broadcast(128)); P.dma(lnb[:], D.ln1_b.partition_broadcast(128))
        P.I("dve", "tensor_scalar", out=lnb[:], in0=lnb[:], scalar1=ALPHA, scalar2=None, op0=ALU.mult)

        def layer_norm_tile(a, g_t, b_t, post_scale):
            P.I("dve", "tensor_reduce", out=sml["s1"][:], in_=a[:], axis=AX.X, op=ALU.add)
            P.I("dve", "tensor_scalar", out=sml["nm"][:], in0=sml["s1"][:], scalar1=-1.0 / 1024.0, scalar2=None, op0=ALU.mult)
            P.I("act", "activation", out=a[:], in_=a[:], func=AF.Identity, bias=sml["nm"][:, 0:1])
            P.I("act", "activation", out=junkf[:], in_=a[:], func=AF.Square, accum_out=sml["ssq"][:])
            P.I("act", "activation", out=sml["sd"][:], in_=sml["ssq"][:], func=AF.Sqrt, scale=1.0 / 1024.0, bias=eps5[:, 0:1])
            P.I("dve", "reciprocal", out=sml["rstd"][:], in_=sml["sd"][:])
            if post_scale != 1.0:
                P.I("dve", "tensor_scalar", out=sml["rstd"][:], in0=sml["rstd"][:], scalar1=post_scale, scalar2=None, op0=ALU.mult)
            P.I("dve", "scalar_tensor_tensor", out=a[:], in0=a[:], scalar=sml["rstd"][:, 0:1], in1=g_t[:], op0=ALU.mult, op1=ALU.mult)
            P.I("dve", "tensor_tensor", out=a[:], in0=a[:], in1=b_t[:], op=ALU.add)

        def router(ti):
            S = sml
            g4 = lambda v: v.rearrange("p (g k) -> p g k", g=4)
            for c in range(8):
                P.I("pe", "matmul", out=bk[2][:, 0:20], lhsT=hf[:, c, :], rhs=wr[:, c, :], start=(c == 0), stop=(c == 7))
            P.I("dve", "tensor_tensor", out=S["lgt"][:], in0=bk[2][:, 0:20], in1=br[:], op=ALU.add)
            P.I("dve", "tensor_reduce", out=S["m"][:], in_=S["lgt"][:, 0:4], axis=AX.X, op=ALU.max)
            P.I("dve", "tensor_scalar", out=S["oh"][:], in0=S["lgt"][:, 0:4], scalar1=S["m"][:, 0:1], scalar2=None, op0=ALU.is_equal)
            P.I("dve", "tensor_scalar", out=S["negm"][:], in0=S["m"][:], scalar1=-1.0, scalar2=None, op0=ALU.mult)
            P.I("act", "activation", out=S["eg"][:], in_=S["lgt"][:, 0:4], func=AF.Exp, bias=S["negm"][:, 0:1], accum_out=S["se"][:])
            P.I("dve", "reciprocal", out=S["gval"][:], in_=S["se"][:])
            P.I("dve", "tensor_tensor", out=g4(S["prod"][:]), in0=g4(S["lgt"][:, 4:20]), in1=S["oh"][:].unsq(2).bcast([128, 4, 4]), op=ALU.mult)
            P.I("dve", "tensor_reduce", out=S["esel"][:], in_=S["prod"][:].rearrange("p (g k) -> p k g", g=4), axis=AX.X, op=ALU.add)
            P.I("dve", "tensor_reduce", out=S["m1"][:], in_=S["esel"][:], axis=AX.X, op=ALU.max)
            P.I("dve", "tensor_scalar", out=S["oh1"][:], in0=S["esel"][:], scalar1=S["m1"][:, 0:1], scalar2=None, op0=ALU.is_equal)
            P.I("dve", "scalar_tensor_tensor", out=S["msk"][:], in0=S["oh1"][:], scalar=-1e30, in1=S["esel"][:], op0=ALU.mult, op1=ALU.add)
            P.I("dve", "tensor_reduce", out=S["m2"][:], in_=S["msk"][:], axis=AX.X, op=ALU.max)
            P.I("dve", "tensor_scalar", out=S["oh2"][:], in0=S["msk"][:], scalar1=S["m2"][:, 0:1], scalar2=None, op0=ALU.is_equal)
            P.I("dve", "tensor_tensor", out=S["d12"][:], in0=S["m2"][:], in1=S["m1"][:], op=ALU.subtract)
            P.I("act", "activation", out=S["e21"][:], in_=S["d12"][:], func=AF.Exp)
            P.I("dve", "tensor_scalar", out=S["den"][:], in0=S["e21"][:], scalar1=1.0, scalar2=None, op0=ALU.add)
            P.I("dve", "reciprocal", out=S["w1"][:], in_=S["den"][:])
            P.I("dve", "tensor_tensor", out=S["w2"][:], in0=S["e21"][:], in1=S["w1"][:], op=ALU.mult)
            P.I("dve", "tensor_scalar", out=S["tq"][:], in0=S["oh1"][:], scalar1=S["w1"][:, 0:1], scalar2=None, op0=ALU.mult)
            P.I("dve", "scalar_tensor_tensor", out=S["wg"][:], in0=S["oh2"][:], scalar=S["w2"][:, 0:1], in1=S["tq"][:], op0=ALU.mult, op1=ALU.add)
            P.I("dve", "tensor_scalar", out=S["ohg"][:], in0=S["oh"][:], scalar1=S["gval"][:, 0:1], scalar2=None, op0=ALU.mult)
            P.I("dve", "tensor_tensor", out=g4(comb[:, ti, :]), in0=S["ohg"][:].unsq(2).bcast([128, 4, 4]), in1=S["wg"][:].unsq(1).bcast([128, 4, 4]), op=ALU.mult)

        def ln1_tile(ti):
            layer_norm_tile(acc[ti], lng, lnb, ALPHA)
            for c in range(8):
                P.I("pe", "transpose", out=bk[c // 4][:, (c % 4) * 128:(c % 4 + 1) * 128], in_=acc[ti][:, c * 128:(c + 1) * 128], identity=ident_f[:])
            for c in range(8):
                src = bk[c // 4][:, (c % 4) * 128:(c % 4 + 1) * 128]
                if c % 2 == 0:
                    P.I("act", "activation", out=hf[:, c, :], in_=src, func=AF.Identity, scale=modT[:, 32 + c, b:b + 1], bias=modT[:, 24 + c, b:b + 1])
                else:
                    P.I("dve", "tensor_scalar", out=hf[:, c, :], in0=src, scalar1=modT[:, 32 + c, b:b + 1], scalar2=modT[:, 24 + c, b:b + 1], op0=ALU.mult, op1=ALU.add)
            P.I("pool", "tensor_copy", out=hT[ti // 4][:, :, (ti % 4) * 128:(ti % 4 + 1) * 128], in_=hf[:])
            router(ti)

        for kh in range(2):
            for kc in range(8):
                st = stage.get()
                P.dma(st[:, 0:1024], D.w_out[(kh * 8 + kc) * 128:(kh * 8 + kc + 1) * 128, :])
                P.I("pool", "tensor_tensor", out=woH[:, kc, :], in0=st[:, 0:1024], in1=gA[:], op=ALU.mult)
            for blk in range(4):
                catb = catb_r.get()
                P.dma(catb[:], cat[kh * 8:(kh + 1) * 8, :, blk * 512:(blk + 1) * 512].rearrange("k p t -> p k t"))
                for tt in range(4):
                    ti = blk * 4 + tt
                    if kh == 0:
                        xt_ = xt.get()
                        P.dma(xt_[:], D.x[b, ti * 128:(ti + 1) * 128, :])
                    for half in range(2):
                        bank = bk[3 + (ti * 2 + half) % 5]
                        hs = slice(half * 512, (half + 1) * 512)
                        for kc in range(8):
                            P.I("pe", "matmul", out=bank[:], lhsT=catb[:, kc, tt * 128:(tt + 1) * 128], rhs=woH[:, kc, hs], start=(kc == 0), stop=(kc == 7))
                        if kh == 0:
                            P.I("dve", "scalar_tensor_tensor", out=acc[ti][:, hs], in0=xt_[:, hs], scalar=ALPHA, in1=bank[:], op0=ALU.mult, op1=ALU.add)
                        else:
                            P.I("dve", "tensor_tensor", out=acc[ti][:, hs], in0=bank[:], in1=acc[ti][:, hs], op=ALU.add)
                    if kh == 1:
                        ln1_tile(ti)
        if b == 0:
            tap("x1a0", acc[0][:], [128, 1024]); tap("comb", comb[:].rearrange("p t e -> p (t e)"), [128, 256])
            tap("hT0", hT[0][:].rearrange("p k t -> p (k t)"), [128, 4096], BF16)
        stop("ln1")
        P.release(mo2)

        lng2 = P.sb("lng2", [128, 1024]); P.dma(lng2[:], D.ln2_g.partition_broadcast(128))
        lnb2 = P.sb("lnb2", [128, 1024]); P.dma(lnb2[:], D.ln2_b.partition_broadcast(128))
        wgb = Ring(P, "wgb", 2, [128, 8, 256], BF16); wub = Ring(P, "wub", 2, [128, 8, 256], BF16); wdb = Ring(P, "wdb", 2, [128, 2, 1024], BF16)
        sgr = Ring(P, "sg", 2, [128, 2, 512], BF16); hidr = Ring(P, "hid", 2, [128, 2, 512], BF16)
        def load_expert(e):
            st = stage.get(); wg_ = wgb.get()
            P.dma(st[:].rearrange("p (k f) -> p k f", k=8), D.w_gate[e].rearrange("(k p) f -> p k f", p=128))
            P.I("pool", "tensor_copy", out=wg_[:].rearrange("p k f -> p (k f)"), in_=st[:])
            st = stage.get(); wu_ = wub.get()
            P.dma(st[:].rearrange("p (k f) -> p k f", k=8), D.w_up[e].rearrange("(k p) f -> p k f", p=128))
            P.I("pool", "tensor_copy", out=wu_[:].rearrange("p k f -> p (k f)"), in_=st[:])
            st = stage.get(); wd_ = wdb.get()
            P.dma(st[:].rearrange("p (k d) -> p k d", k=2), D.w_down[e].rearrange("(k p) d -> p k d", p=128))
            for k in range(2):
                P.I("pool", "tensor_tensor", out=wd_[:, k, :], in0=st[:, k * 1024:(k + 1) * 1024], in1=gF[:], op=ALU.mult)
            return wg_, wu_, wd_

        wts = {0: load_expert(0)}
        items = [(e, blk) for e in range(16) for blk in range(4)]
        hids = {}

        def gate_up(i):
            e, blk = items[i]
            if blk == 1 and e + 1 < 16:
                wts[e + 1] = load_expert(e + 1)
            wg_, wu_, _ = wts[e]
            for fc in range(2):
                for kc in range(8):
                    P.I("pe", "matmul", out=bk[2 * fc][:], lhsT=wg_[:, kc, fc * 128:(fc + 1) * 128], rhs=hT[blk][:, kc, :], start=(kc == 0), stop=(kc == 7))
                for kc in range(8):
                    P.I("pe", "matmul", out=bk[2 * fc + 1][:], lhsT=wu_[:, kc, fc * 128:(fc + 1) * 128], rhs=hT[blk][:, kc, :], start=(kc == 0), stop=(kc == 7))
            sg = sgr.get(); hid = hidr.get()
            for fc in range(2):
                P.I("act", "activation", out=sg[:, fc, :], in_=bk[2 * fc][:], func=AF.Silu)
                P.I("dve", "tensor_tensor", out=hid[:, fc, :], in0=bk[2 * fc + 1][:], in1=sg[:, fc, :], op=ALU.mult)
            hids[i] = hid

        def down(i):
            e, blk = items[i]
            hid = hids.pop(i); wd_ = wts[e][2]
            for tt in range(4):
                ti = blk * 4 + tt
                for half in range(2):
                    bank = bk[4 + (tt * 2 + half) % 4]
                    hs = slice(half * 512, (half + 1) * 512)
                    for fc in range(2):
                        P.I("pe", "matmul", out=bank[:], lhsT=hid[:, fc, tt * 128:(tt + 1) * 128], rhs=wd_[:, fc, hs], start=(fc == 0), stop=(fc == 1))
                    P.I("dve", "scalar_tensor_tensor", out=acc[ti][:, hs], in0=bank[:], scalar=comb[:, ti, e:e + 1], in1=acc[ti][:, hs], op0=ALU.mult, op1=ALU.add)
                if e == 15:
                    layer_norm_tile(acc[ti], lng2, lnb2, 1.0)
                    P.dma(ybuf[b, ti * 128:(ti + 1) * 128, :], acc[ti][:])

        gate_up(0)
        for i in range(len(items)):
            if i + 1 < len(items):
                gate_up(i + 1)
            down(i)
        if b == 0:
            tap("pre2", acc[0][:], [128, 1024])
        stop("moe")
        P.release(mb)

    P.barrier()
    return nc, P


def finish(nc, P):
    P.barrier()
    P.build()
    return nc


def _const_tables():
    i = np.arange(128)
    le = (i[:, None] <= i[None, :]).astype(np.float32)
    ge = (i[:, None] >= i[None, :]).astype(np.float32)
    gt = (i[:, None] > i[None, :]).astype(np.float32)
    lt = (i[:, None] < i[None, :]).astype(np.float32)
    tri = np.stack([le, ge, gt, lt, np.ones((128, 128), np.float32)])
    prot = np.zeros((128, 128), np.float32)
    for j in range(64):
        prot[2 * j + 1, 2 * j] = -1.0
        prot[2 * j, 2 * j + 1] = 1.0
    bones = (i[:, None] // 64 == i[None, :] // 64).astype(np.float32)
    t = np.arange(T)
    rows = (t // 64).astype(np.float32)
    cols = (t % 64).astype(np.float32)
    inv_freq = np.power(np.float32(10000.0), -np.arange(0, 32, 2, dtype=np.float32) / np.float32(32)).astype(np.float32)
    ang = np.concatenate([rows[:, None] * inv_freq, cols[:, None] * inv_freq], -1).astype(np.float32)
    pair = (i % 64) // 2
    return {"c_ident": np.eye(128, dtype=np.float32), "c_tri": tri, "c_prot": prot, "c_bones": bones,
            "c_cos": np.ascontiguousarray(np.cos(ang)[:, pair].T.astype(np.float32)),
            "c_sin": np.ascontiguousarray(np.sin(ang)[:, pair].T.astype(np.float32))}


def _core_inputs(d, core, consts):
    b0 = core * NB
    f = lambda a: np.ascontiguousarray(np.asarray(a, dtype=np.float32))
    m = {"x": d["x"][b0:b0 + NB], "ctx": d["ctx"][b0:b0 + NB],
         "crow": np.concatenate([np.asarray(d["c"])[b0:b0 + NB], np.asarray(d["c_ctx"])[None]], 0),
         "w_mod": d["w_mod"][0], "b_mod": d["b_mod"], "w_in": d["w_in"][0], "conv_w": d["conv_w"][0], "conv_b": d["conv_b"],
         "dt_bias": np.asarray(d["dt_bias"]).reshape(1, 32), "a_log": np.asarray(d["a_log"]).reshape(1, 32), "d_skip": d["d_skip"],
         "ssd_norm_g": d["ssd_norm_g"], "q_norm_g": np.asarray(d["q_norm_g"]).reshape(64, 1), "k_norm_g": np.asarray(d["k_norm_g"]).reshape(64, 1),
         "w_out": d["w_out"][0], "ln1_g": d["ln1_g"], "ln1_b": d["ln1_b"], "ln2_g": d["ln2_g"], "ln2_b": d["ln2_b"],
         "w_r": np.concatenate([np.asarray(d["w_rg"])[0], np.asarray(d["w_re"])[0]], 1),
         "b_r": np.concatenate([np.asarray(d["b_rg"]), np.asarray(d["b_re"])], 1),
         "w_gate": d["w_gate"][0], "w_up": d["w_up"][0], "w_down": d["w_down"][0]}
    m.update(consts)
    return {k: f(v) for k, v in m.items()}


def kernel(**inputs):
    n_cores = 8
    nc, P = build_program()
    finish(nc, P)
    consts = _const_tables()
    shared = None
    in_maps = []
    for core in range(n_cores):
        ci = _core_inputs(inputs, core, consts)
        if shared is None:
            shared = ci
        else:
            for k in ci:
                if k not in ("x", "ctx", "crow"):
                    ci[k] = shared[k]
        in_maps.append({k: ci[k] for k in used_inputs})
    res = run_bass_kernel_spmd(nc, in_maps, core_ids=list(range(n_cores)))
    return np.concatenate([np.asarray(r["y"]) for r in res.results], axis=0).astype(np.float32)
```

```python
import numpy as np
import concourse.bass as bass
import concourse.mybir as mybir
from concourse.bass_utils import run_bass_kernel_spmd

F32 = mybir.dt.float32
BF16 = mybir.dt.bfloat16
AF = mybir.ActivationFunctionType
ALU = mybir.AluOpType
AX = mybir.AxisListType

SB_BASE = 16512
SB_END = 229376


class Buf:
    def __init__(self, t, name):
        self.t = t
        self.name = name
        self.w = None
        self.r = {}
        self.dsem = None
        self.excl = False

    def __getitem__(self, k):
        return View(self, self.t[k])


class View:
    def __init__(self, buf, ap):
        self.buf = buf
        self.ap = ap

    def __getitem__(self, k):
        return View(self.buf, self.ap[k])

    def rearrange(self, s, **kw):
        return View(self.buf, self.ap.rearrange(s, **kw))

    def bcast(self, shape):
        return View(self.buf, self.ap.broadcast_to(list(shape)))

    def unsq(self, axis):
        return View(self.buf, self.ap.unsqueeze(axis))

    def bitcast(self, dt):
        return View(self.buf, self.ap.bitcast(dt))

    @property
    def shape(self):
        return tuple(self.ap.shape)


WRITE_KEYS = ("out", "accum_out", "ap")


class Prog:
    ENGS = ("pe", "act", "dve", "pool", "sp")

    def __init__(self, nc):
        self.nc = nc
        self.sems = {}
        self.cnt = {}
        self.seen = {e: {} for e in self.ENGS}
        self.streams = {e: [] for e in self.ENGS}
        self.top = SB_BASE
        self.ninst = 0
        for e in self.ENGS:
            self._mksem(e)

    def _mksem(self, name):
        self.sems[name] = self.nc.alloc_semaphore(name)
        self.cnt[name] = 0

    def sb(self, name, shape, dtype=F32):
        nbytes = int(np.prod(shape[1:])) * (4 if dtype == F32 else 2)
        nbytes = (nbytes + 63) // 64 * 64
        off = self.top
        assert off + nbytes <= SB_END, f"SBUF overflow allocating {name}: {off + nbytes - SB_BASE}"
        self.top += nbytes
        self.maxtop = max(getattr(self, 'maxtop', 0), self.top)
        uname = f"{name}_{off}"
        self.nalloc = getattr(self, "nalloc", 0) + 1
        return Buf(self.nc.alloc_sbuf_tensor_at(f"{uname}_{self.nalloc}", list(shape), dtype, offset=off), uname)

    def mark(self):
        return self.top

    def release(self, mark):
        self.barrier()
        self.top = mark

    def ps(self, name, shape, dtype=F32):
        b = Buf(self.nc.alloc_psum_tensor(name, list(shape), dtype), name)
        b.excl = True
        return b

    def dram(self, name, shape, dtype, kind="Internal"):
        return Buf(self.nc.dram_tensor(name, list(shape), dtype, kind=kind).ap(), name)

    @staticmethod
    def _add(deps, tok):
        if tok is None:
            return
        s, v = tok
        if deps.get(s, 0) < v:
            deps[s] = v

    def _deps(self, reads, writes):
        deps = {}
        for b in reads:
            self._add(deps, b.w)
        for b in writes:
            self._add(deps, b.w)
            for s, v in b.r.items():
                self._add(deps, (s, v))
        return deps

    def _emit_waits(self, e, deps, skip_self):
        seen = self.seen[e]
        for s, v in deps.items():
            if s == e and skip_self:
                continue
            if seen.get(s, 0) >= v:
                continue
            seen[s] = v
            sem = self.sems[s]
            self.streams[e].append(lambda eng, sem=sem, v=v: eng.wait_ge(sem, v))

    def _mark(self, tok, reads, writes):
        s, v = tok
        for b in reads:
            if b.r.get(s, 0) < v:
                b.r[s] = v
        for b in writes:
            b.w = tok
            b.r = {}

    @staticmethod
    def _split(kw):
        reads, writes, real = [], [], {}
        for k, a in kw.items():
            if isinstance(a, View):
                (writes if (k in WRITE_KEYS or a.buf.excl) else reads).append(a.buf)
                real[k] = a.ap
            else:
                real[k] = a
        return reads, writes, real

    def I(self, e, method, **kw):
        reads, writes, real = self._split(kw)
        if method == "matmul" and kw.get("start") is False:
            reads = reads + writes
        deps = self._deps(reads, writes)
        self._emit_waits(e, deps, skip_self=(e == "pe"))
        self.cnt[e] += 1
        sem = self.sems[e]
        self.streams[e].append(lambda eng, m=method, real=real, sem=sem: getattr(eng, m)(**real).then_inc(sem, 1))
        self._mark((e, self.cnt[e]), reads, writes)
        self.ninst += 1

    def dma(self, out, in_, q="sp", **kw):
        reads, writes = [], []
        if isinstance(in_, View):
            reads.append(in_.buf)
            in_ = in_.ap
        if isinstance(out, View):
            writes.append(out.buf)
            out = out.ap
        deps = self._deps(reads, writes)
        self._emit_waits(q, deps, skip_self=True)
        tb = writes[0] if writes else reads[0]
        if tb.dsem is None:
            tb.dsem = "d_" + tb.name
            if tb.dsem not in self.sems:
                self._mksem(tb.dsem)
        s = tb.dsem
        self.cnt[s] += 16
        sem = self.sems[s]
        self.streams[q].append(lambda eng, out=out, in_=in_, sem=sem, kw=kw: eng.dma_start(out=out, in_=in_, **kw).then_inc(sem, 16))
        self._mark((s, self.cnt[s]), reads, writes)
        self.ninst += 1

    def barrier(self):
        for e in self.ENGS:
            deps = {s: v for s, v in self.cnt.items() if v > 0}
            self._emit_waits(e, deps, skip_self=(e == "pe"))

    def build(self):
        with self.nc.Block() as block:
            def mk(e):
                def run(eng):
                    for f in self.streams[e]:
                        f(eng)
                return run
            block.tensor(mk("pe"))
            block.scalar(mk("act"))
            block.vector(mk("dve"))
            block.gpsimd(mk("pool"))
            block.sync(mk("sp"))


class Ring:
    def __init__(self, P, name, n, shape, dtype=F32):
        self.bufs = [P.sb(f"{name}{i}", shape, dtype) for i in range(n)]
        self.i = 0

    def get(self):
        b = self.bufs[self.i % len(self.bufs)]
        self.i += 1
        return b


OFF_Z, OFF_Q, OFF_XBC, OFF_DT, OFF_K, OFF_V, D_IN = 0, 1024, 2048, 3584, 3616, 3872, 4128
T = 2048
TC = 256
ALPHA = 2.0 ** 0.25
NB = 2


class StopBuild(Exception):
    pass


_last = [None, None]
used_inputs = []


def build_program(dbg=(), stop_after=None, nb=NB, skip=()):
    nc = bass.Bass("TRN2", target_bir_lowering=False)
    P = Prog(nc)
    _last[:] = [nc, P]

    in_shapes = {
        "x": [NB, T, 1024], "ctx": [NB, TC, 1024], "crow": [3, 1024], "w_mod": [1024, 6144], "b_mod": [1, 6144],
        "w_in": [1024, D_IN], "conv_w": [5, 1536], "conv_b": [1, 1536], "dt_bias": [1, 32], "a_log": [1, 32], "d_skip": [1, 16],
        "ssd_norm_g": [1, 1024], "q_norm_g": [64, 1], "k_norm_g": [64, 1], "w_out": [2048, 1024],
        "ln1_g": [1, 1024], "ln1_b": [1, 1024], "ln2_g": [1, 1024], "ln2_b": [1, 1024], "w_r": [1024, 20], "b_r": [1, 20],
        "w_gate": [16, 1024, 256], "w_up": [16, 1024, 256], "w_down": [16, 256, 1024],
        "c_ident": [128, 128], "c_tri": [5, 128, 128], "c_prot": [128, 128], "c_bones": [128, 128], "c_cos": [128, T], "c_sin": [128, T],
    }
    in_aps = {}
    used_inputs.clear()

    class _D:
        def __getattr__(self, name):
            if name not in in_aps:
                in_aps[name] = nc.dram_tensor(name, list(in_shapes[name]), F32, kind="ExternalInput").ap()
                used_inputs.append(name)
            return in_aps[name]
    D = _D()
    y_d = nc.dram_tensor("y", [NB, T, 1024], F32, kind="ExternalOutput").ap()
    cat = P.dram("cat", [16, 128, T], BF16)
    ybuf = Buf(y_d, "ybuf")

    def tap(name, view, shape, dt=F32):
        if name in dbg:
            o = nc.dram_tensor("dbg_" + name, list(shape), dt, kind="ExternalOutput").ap()
            P.dma(o, view)

    def stop(name):
        if stop_after == name:
            raise StopBuild()

    bk = [P.ps(f"bank{i}", [128, 512]) for i in range(4)]
    spair = []
    for i in range(2):
        pt_ = nc.alloc_psum_tensor(f"pp{i}", [128, 1024], F32)
        for vw in (pt_[:, 0:512], pt_[:, 512:1024]):
            b_ = Buf(vw, f"bank{len(bk)}"); b_.excl = True; bk.append(b_)
        b_ = Buf(pt_[:, :], f"spair{i}"); b_.excl = True; spair.append(b_)

    ident_f = P.sb("ident_f", [128, 128]); ident_b = P.sb("ident_b", [128, 128], BF16)
    tri = P.sb("tri", [128, 5, 128])
    TRI_LE, TRI_GE, TRI_GT, TRI_LT, TRI_ONE = range(5)
    prot_b = P.sb("prot_b", [128, 128], BF16); bones_b = P.sb("bones_b", [128, 128], BF16)
    convw = P.sb("convw", [128, 12, 5]); convb = P.sb("convb", [128, 12])
    gq = P.sb("gq", [128, 1]); gk = P.sb("gk", [128, 1])
    ssdg = P.sb("ssdg", [128, 1024]); dskip = P.sb("dskip", [128, 16]); dtb = P.sb("dtb", [128, 32]); Aneg = P.sb("Aneg", [128, 32])
    br = P.sb("br", [128, 20]); wr = P.sb("wr", [128, 8, 20])
    modT = P.sb("modT", [128, 48, 3])
    gA = P.sb("gA", [128, 1024]); gF = P.sb("gF", [128, 1024])
    eps5 = P.sb("eps5", [128, 1]); eps6 = P.sb("eps6", [128, 1])
    stage = Ring(P, "stage", 2, [128, 2048])
    xt = Ring(P, "xt", 4, [128, 1024])

    P.dma(ident_f[:], D.c_ident)
    P.dma(tri[:], D.c_tri.rearrange("k p q -> p k q"))
    P.I("pool", "tensor_copy", out=ident_b[:], in_=ident_f[:])
    s0 = stage.get()
    P.dma(s0[:, 0:128], D.c_prot); P.dma(s0[:, 128:256], D.c_bones)
    P.I("pool", "tensor_copy", out=prot_b[:], in_=s0[:, 0:128])
    P.I("pool", "tensor_copy", out=bones_b[:], in_=s0[:, 128:256])
    for c in range(12):
        P.dma(convw[:, c, :], D.conv_w[:, c * 128:(c + 1) * 128].rearrange("k p -> p k"), allow_slow_non_contiguous=True)
        P.dma(convb[:, c:c + 1], D.conv_b[:, c * 128:(c + 1) * 128].rearrange("k p -> p k"), allow_slow_non_contiguous=True)
    for h in range(2):
        P.dma(gq[h * 64:(h + 1) * 64, :], D.q_norm_g); P.dma(gk[h * 64:(h + 1) * 64, :], D.k_norm_g)
    P.dma(ssdg[:], D.ssd_norm_g.partition_broadcast(128)); P.dma(dskip[:], D.d_skip.partition_broadcast(128))
    P.dma(dtb[:], D.dt_bias.partition_broadcast(128)); P.dma(Aneg[:], D.a_log.partition_broadcast(128))
    P.dma(br[:], D.b_r.partition_broadcast(128))
    P.dma(wr[:], D.w_r.rearrange("(k p) n -> p k n", p=128))
    P.I("act", "activation", out=Aneg[:], in_=Aneg[:], func=AF.Exp)
    P.I("dve", "tensor_scalar", out=Aneg[:], in0=Aneg[:], scalar1=-1.0, scalar2=None, op0=ALU.mult)
    P.I("dve", "memset", ap=eps5[:], constant=1e-5); P.I("dve", "memset", ap=eps6[:], constant=1e-6)

    m0 = P.mark()
    crow = P.sb("crow", [3, 1024]); scT = P.sb("scT", [128, 8, 3]); bmod3 = P.sb("bmod3", [3, 6144]); modrow = P.sb("modrow", [3, 6144])
    P.dma(crow[:], D.crow)
    P.dma(bmod3[:], D.b_mod.partition_broadcast(3))
    P.I("act", "activation", out=crow[:], in_=crow[:], func=AF.Silu)
    for kc in range(8):
        P.I("pe", "transpose", out=bk[0][:, kc * 3:(kc + 1) * 3], in_=crow[0:3, kc * 128:(kc + 1) * 128], identity=ident_f[0:3, 0:3])
    P.I("dve", "tensor_copy", out=scT[:].rearrange("p k r -> p (k r)"), in_=bk[0][:, 0:24])
    wm_r = Ring(P, "wmst", 6, [128, 2048])
    for third in range(3):
        for kc in range(8):
            st = wm_r.get()
            P.dma(st[:], D.w_mod[kc * 128:(kc + 1) * 128, third * 2048:(third + 1) * 2048])
            for j in range(4):
                P.I("pe", "matmul", out=bk[1 + j][0:3, :], lhsT=scT[:, kc, :], rhs=st[:, j * 512:(j + 1) * 512], start=(kc == 0), stop=(kc == 7))
        for j in range(4):
            c0 = third * 2048 + j * 512
            P.I("dve", "tensor_tensor", out=modrow[:, c0:c0 + 512], in0=bk[1 + j][0:3, :], in1=bmod3[:, c0:c0 + 512], op=ALU.add)
    for j in range(48):
        P.I("pe", "transpose", out=bk[0][:, j * 3:(j + 1) * 3], in_=modrow[0:3, j * 128:(j + 1) * 128], identity=ident_f[0:3, 0:3])
    P.I("dve", "tensor_copy", out=modT[:].rearrange("p k r -> p (k r)"), in_=bk[0][:, 0:144])
    P.I("dve", "tensor_scalar", out=modT[:, 8:16, :], in0=modT[:, 8:16, :], scalar1=1.0, scalar2=None, op0=ALU.add)
    P.I("dve", "tensor_scalar", out=modT[:, 32:40, :], in0=modT[:, 32:40, :], scalar1=1.0, scalar2=1.0 / ALPHA, op0=ALU.add, op1=ALU.mult)
    tap("modT", modT[:].rearrange("p k r -> p (k r)"), [128, 144])
    P.release(m0)
    stop("mod")

    def bcast_row(dst, sec, row):
        rep = stage.get()
        for c in range(8):
            P.I("dve", "tensor_copy", out=rep[:, c * 128:(c + 1) * 128], in_=modT[:, sec * 8 + c, row:row + 1].bcast([128, 128]))
        for c in range(8):
            P.I("pe", "matmul", out=bk[c // 4][:, (c % 4) * 128:(c % 4 + 1) * 128], lhsT=rep[:, c * 128:(c + 1) * 128], rhs=ident_f[:], start=True, stop=True)
        for hh in range(2):
            P.I("act", "activation", out=dst[:, hh * 512:(hh + 1) * 512], in_=bk[hh][:], func=AF.Identity)

    for b in range(nb):
        mb = P.mark()
        bcast_row(gA, 2, b)
        bcast_row(gF, 5, b)
        tap(f"gA{b}", gA[:], [128, 1024])
        hxT = P.sb("hxT", [128, 8, T + 4], BF16)
        hcT = P.sb("hcT", [128, 8, TC + 4], BF16)
        P.I("pool", "memset", ap=hxT[:, :, 0:2], constant=0.0); P.I("pool", "memset", ap=hxT[:, :, T + 2:T + 4], constant=0.0)
        P.I("pool", "memset", ap=hcT[:, :, 0:2], constant=0.0); P.I("pool", "memset", ap=hcT[:, :, TC + 2:TC + 4], constant=0.0)

        def build_hT(dstT, src_d, ntile, row):
            for blk in range((ntile + 3) // 4):
                nt = min(4, ntile - blk * 4)
                tiles = []
                for tt in range(nt):
                    t_ = xt.get()
                    P.dma(t_[:], src_d[(blk * 4 + tt) * 128:(blk * 4 + tt + 1) * 128, :])
                    tiles.append(t_)
                for c in range(8):
                    bank = bk[c % 4]
                    for tt in range(nt):
                        P.I("pe", "transpose", out=bank[:, tt * 128:(tt + 1) * 128], in_=tiles[tt][:, c * 128:(c + 1) * 128], identity=ident_f[:])
                    dst = dstT[:, c, 2 + blk * 512:2 + blk * 512 + nt * 128]
                    if c % 2 == 0:
                        P.I("act", "activation", out=dst, in_=bank[:, 0:nt * 128], func=AF.Identity, scale=modT[:, 8 + c, row:row + 1], bias=modT[:, c, row:row + 1])
                    else:
                        P.I("dve", "tensor_scalar", out=dst, in0=bank[:, 0:nt * 128], scalar1=modT[:, 8 + c, row:row + 1], scalar2=modT[:, c, row:row + 1], op0=ALU.mult, op1=ALU.add)

        build_hT(hcT, D.ctx[b], 2, 2)
        build_hT(hxT, D.x[b], 16, b)
        if b == 0:
            tap("hxT", hxT[:].rearrange("p k t -> p (k t)"), [128, 8 * (T + 4)], BF16)
            tap("hcT", hcT[:].rearrange("p k t -> p (k t)"), [128, 8 * (TC + 4)], BF16)
        stop("hx")

        def load_cols(dst_w, col_specs):
            for kc in range(8):
                st = stage.get()
                o = 0
                for (c0, n) in col_specs:
                    P.dma(st[:, o:o + n], D.w_in[kc * 128:(kc + 1) * 128, c0:c0 + n])
                    o += n
                P.I("pool", "tensor_copy", out=dst_w[:, kc, 0:o], in_=st[:, 0:o])

        hv = lambda v: v.rearrange("p (h j) -> p h j", h=8)

        def ssd_pass(g):
            mp = P.mark()
            wZ = P.sb("wZ", [128, 8, 512], BF16)
            cidx = [g * 4, g * 4 + 1, g * 4 + 2, g * 4 + 3, 8 + g, 10 + g]

            dtA = P.sb("dtA", [128, 18, 16]); aA = P.sb("aA", [128, 18, 16])
            ewA = [P.sb("ewF", [128, 18, 16]), P.sb("ewB", [128, 18, 16])]

            def mkstore(tag, n, i0):
                return [dict(xs=P.sb(f"xs{tag}{i}", [128, 512], BF16), Bt=P.sb(f"Bt{tag}{i}", [128, 128], BF16),
                             BT=P.sb(f"BT{tag}{i}", [128, 128], BF16), CT=P.sb(f"CT{tag}{i}", [128, 128], BF16),
                             dt=dtA[:, i0 + i, :], a=aA[:, i0 + i, :], idx=i0 + i) for i in range(n)]
            lat = mkstore("L", 16, 0)
            hin_b = [P.sb(f"hinb{i}", [128, 512], BF16) for i in range(16)]
            hin_f = Ring(P, "hinf", 3, [128, 512], BF16)
            h_f = P.sb("h_f", [128, 512]); h_b = P.sb("h_b", [128, 512])
            sm_r = {n: Ring(P, "sm_" + n, 2, [128, 16]) for n in ("sc", "ecol")}
            xw_r = Ring(P, "xw", 2, [128, 512], BF16)

            def update(h, st, dirn, bank_s):
                sc = sm_r["sc"].get(); xw = xw_r.get()
                ew = ewA[dirn][:, st["idx"], :]
                P.I("dve", "tensor_tensor", out=sc[:, 0:8], in0=st["dt"][:, dirn * 8:(dirn + 1) * 8], in1=ew[:, 0:8], op=ALU.mult)
                P.I("dve", "tensor_tensor", out=hv(xw[:]), in0=hv(st["xs"][:]), in1=sc[:, 0:8].unsq(2).bcast([128, 8, 64]), op=ALU.mult)
                P.I("pe", "matmul", out=bank_s[:], lhsT=st["Bt"][:], rhs=xw[:], start=True, stop=True)
                P.I("pool", "tensor_tensor", out=hv(h[:]), in0=hv(h[:]), in1=ew[:, 8:16].unsq(2).bcast([128, 8, 64]), op=ALU.mult)
                P.I("dve", "tensor_tensor", out=h[:], in0=bank_s[:], in1=h[:], op=ALU.add)

            m1 = P.mark()
            wS = P.sb("wS", [128, 8, 784], BF16)
            load_cols(wS, [(OFF_XBC + g * 512, 512), (OFF_XBC + 1024 + g * 128, 128), (OFF_XBC + 1280 + g * 128, 128),
                           (OFF_DT + g * 8, 8), (OFF_DT + 16 + g * 8, 8)])
            load_cols(wZ, [(OFF_Z + g * 512, 512)])
            cst = mkstore("C", 2, 16)
            rawS_r = Ring(P, "rawS", 2, [128, 6, 132], BF16); xcT_r = Ring(P, "xcT", 2, [128, 4, 128], BF16)
            dg = P.sb("dg", [128, 6, 5, 128], BF16)
            for r in range(6):
                for k in range(5):
                    P.I("dve", "tensor_scalar", out=dg[:, r, k, :], in0=ident_f[:], scalar1=convw[:, cidx[r], k:k + 1], scalar2=None, op0=ALU.mult)

            def prep(hT, c, st, need_C):
                rawS = rawS_r.get(); xcT = xcT_r.get()
                tok0 = c * 128
                nr = 6 if need_C else 5
                for r in range(nr):
                    bank = bk[r // 3]
                    o = (r % 3) * 132
                    for kc in range(8):
                        P.I("pe", "matmul", out=bank[:, o:o + 132], lhsT=wS[:, kc, r * 128:(r + 1) * 128], rhs=hT[:, kc, tok0:tok0 + 132], start=(kc == 0), stop=(kc == 7))
                P.I("act", "activation", out=rawS[:, 0:3, :].rearrange("p r t -> p (r t)"), in_=bk[0][:, 0:396], func=AF.Identity)
                P.I("act", "activation", out=rawS[:, 3:nr, :].rearrange("p r t -> p (r t)"), in_=bk[1][:, 0:(nr - 3) * 132], func=AF.Identity)
                for r in range(nr):
                    cb = bk[2][:, r * 128:(r + 1) * 128] if r < 4 else bk[3][:, (r - 4) * 128:(r - 3) * 128]
                    for k in range(5):
                        P.I("pe", "matmul", out=cb, lhsT=dg[:, r, k, :], rhs=rawS[:, r, k:k + 128], start=(k == 0), stop=(k == 4))
                for r in range(nr):
                    cb = bk[2][:, r * 128:(r + 1) * 128] if r < 4 else bk[3][:, (r - 4) * 128:(r - 3) * 128]
                    dst = xcT[:, r, :] if r < 4 else (st["BT"][:] if r == 4 else st["CT"][:])
                    P.I("act", "activation", out=dst, in_=cb, func=AF.Silu, bias=convb[:, cidx[r]:cidx[r] + 1])
                for r in range(4):
                    P.I("pe", "matmul", out=bk[6][:, r * 128:(r + 1) * 128], lhsT=xcT[:, r, :], rhs=ident_b[:], start=True, stop=True)
                P.I("pe", "matmul", out=bk[7][:, 0:128], lhsT=st["BT"][:], rhs=ident_b[:], start=True, stop=True)
                P.I("act", "activation", out=st["xs"][:], in_=bk[6][:], func=AF.Identity)
                P.I("dve", "tensor_copy", out=st["Bt"][:], in_=bk[7][:, 0:128])

            big = {n: P.sb("big_" + n, [128, 18, 16]) for n in ("u", "nu", "na", "e", "l")}
            for idx in range(18):
                hT_, tok0_ = (hxT, idx * 128) if idx < 16 else (hcT, (idx - 16) * 128)
                for kc in range(8):
                    P.I("pe", "matmul", out=bk[3][:, idx * 16:(idx + 1) * 16], lhsT=hT_[:, kc, 2 + tok0_:2 + tok0_ + 128], rhs=wS[:, kc, 768:784], start=(kc == 0), stop=(kc == 7))
            d4 = lambda v: v.rearrange("p i (d h) -> p i d h", d=2)
            fl = lambda v: v.rearrange("p i x -> p (i x)")
            dvg = lambda v: v.rearrange("p (d h) -> p d h", d=2)[:, :, g * 8:(g + 1) * 8].unsq(1).bcast([128, 18, 2, 8])
            P.I("dve", "tensor_tensor", out=d4(big["u"][:]), in0=d4(bk[3][:, 0:288].rearrange("p (i x) -> p i x", i=18)), in1=dvg(dtb[:]), op=ALU.add)
            P.I("dve", "tensor_scalar", out=fl(big["nu"][:]), in0=fl(big["u"][:]), scalar1=-1.0, scalar2=None, op0=ALU.mult)
            P.I("dve", "tensor_tensor", out=fl(big["na"][:]), in0=fl(big["u"][:]), in1=fl(big["nu"][:]), op=ALU.min)
            P.I("act", "activation", out=fl(big["e"][:]), in_=fl(big["na"][:]), func=AF.Exp)
            P.I("act", "activation", out=fl(big["l"][:]), in_=fl(big["e"][:]), func=AF.Ln, bias=1.0, scale=1.0)
            P.I("dve", "scalar_tensor_tensor", out=fl(dtA[:]), in0=fl(big["u"][:]), scalar=0.0, in1=fl(big["l"][:]), op0=ALU.max, op1=ALU.add)
            P.I("dve", "tensor_tensor", out=d4(aA[:]), in0=d4(dtA[:]), in1=dvg(Aneg[:]), op=ALU.mult)
            for dirn in range(2):
                for idx in range(18):
                    a_d = aA[:, idx, dirn * 8:(dirn + 1) * 8]
                    P.I("pe", "matmul", out=bk[4 + dirn][:, idx * 16:idx * 16 + 8], lhsT=tri[:, TRI_GT if dirn == 0 else TRI_LT, :], rhs=a_d, start=True, stop=True)
                    P.I("pe", "matmul", out=bk[4 + dirn][:, idx * 16 + 8:idx * 16 + 16], lhsT=tri[:, TRI_ONE, :], rhs=a_d, start=True, stop=True)
                P.I("act", "activation", out=fl(ewA[dirn][:]), in_=bk[4 + dirn][:, 0:288], func=AF.Exp)

            for cc in range(2):
                prep(hcT, cc, cst[cc], False)
            P.I("pool", "memset", ap=h_f[:], constant=0.0); P.I("pool", "memset", ap=h_b[:], constant=0.0)
            prep(hxT, 15, lat[15], True)
            update(h_f, cst[0], 0, bk[5]); update(h_f, cst[1], 0, bk[5])
            update(h_b, cst[1], 1, bk[5]); update(h_b, cst[0], 1, bk[5])
            if b == 0 and g == 0:
                tap("h0f", h_f[:], [128, 512]); tap("h0b", h_b[:], [128, 512])
            for c in range(15, -1, -1):
                if c > 0:
                    prep(hxT, c - 1, lat[c - 1], True)
                P.I("pool", "tensor_copy", out=hin_b[c][:], in_=h_b[:])
                update(h_b, lat[c], 1, bk[4 + c % 2])
            stop("ssd_s1")
            P.release(m1)

            zs_r = Ring(P, "zs", 2, [128, 512]); CBmF_r = Ring(P, "CBmF", 2, [128, 128]); CBmB_r = Ring(P, "CBmB", 2, [128, 128])
            yoffF_r = Ring(P, "yoffF", 2, [128, 512], BF16); yoffB_r = Ring(P, "yoffB", 2, [128, 512], BF16); xskip_r = Ring(P, "xskip", 2, [128, 512], BF16)
            arepm_r = Ring(P, "arepm", 2, [128, 8, 128]); xdt_r = Ring(P, "xdt", 4, [128, 512], BF16); Lt_r = Ring(P, "Lt", 2, [128, 512])
            Mt_r = Ring(P, "Mt", 8, [128, 4, 128], BF16)
            yz_r = Ring(P, "yz", 2, [128, 512]); junk = P.sb("junk", [128, 512], BF16)
            ssq_r = Ring(P, "ssq", 2, [128, 1]); sd_r = Ring(P, "sd", 2, [128, 1]); rstd_r = Ring(P, "rstd", 2, [128, 1])
            yn_r = Ring(P, "yn", 2, [128, 512], BF16); ynT = P.sb("ynT", [128, 4, 512], BF16)

            def mainA(c):
                st = lat[c]
                A = dict(zs=zs_r.get(), CBm=[CBmF_r.get(), CBmB_r.get()], ecol=sm_r["ecol"].get(), xskip=xskip_r.get(), xdt=[], Mt=[])
                for kc in range(8):
                    P.I("pe", "matmul", out=bk[0][:], lhsT=hxT[:, kc, 2 + c * 128:2 + (c + 1) * 128], rhs=wZ[:, kc, :], start=(kc == 0), stop=(kc == 7))
                P.I("act", "activation", out=A["zs"][:], in_=bk[0][:], func=AF.Silu)
                P.I("pe", "matmul", out=bk[1][:, 0:128], lhsT=st["BT"][:], rhs=st["CT"][:], start=True, stop=True)
                P.I("pe", "matmul", out=bk[1][:, 128:136], lhsT=tri[:, TRI_LE, :], rhs=st["a"][:, 0:8], start=True, stop=True)
                P.I("pe", "matmul", out=bk[1][:, 136:144], lhsT=tri[:, TRI_GE, :], rhs=st["a"][:, 8:16], start=True, stop=True)
                P.I("dve", "tensor_tensor", out=A["CBm"][0][:], in0=bk[1][:, 0:128], in1=tri[:, TRI_LE, :], op=ALU.mult)
                P.I("dve", "tensor_tensor", out=A["CBm"][1][:], in0=bk[1][:, 0:128], in1=tri[:, TRI_GE, :], op=ALU.mult)
                P.I("act", "activation", out=A["ecol"][:], in_=bk[1][:, 128:144], func=AF.Exp)
                P.I("pool", "tensor_tensor", out=hv(A["xskip"][:]), in0=hv(st["xs"][:]), in1=dskip[:, g * 8:(g + 1) * 8].unsq(2).bcast([128, 8, 64]), op=ALU.mult)
                for dirn in range(2):
                    a_d = st["a"][:, dirn * 8:(dirn + 1) * 8]
                    arepm = arepm_r.get(); xdt = xdt_r.get()
                    A["xdt"].append(xdt)
                    P.I("dve", "tensor_tensor", out=arepm[:], in0=a_d.unsq(2).bcast([128, 8, 128]),
                        in1=tri[:, TRI_GT if dirn == 0 else TRI_LT, :].unsq(1).bcast([128, 8, 128]), op=ALU.mult)
                    P.I("pool", "tensor_tensor", out=hv(xdt[:]), in0=hv(st["xs"][:]), in1=st["dt"][:, dirn * 8:(dirn + 1) * 8].unsq(2).bcast([128, 8, 64]), op=ALU.mult)
                    for hb in range(2):
                        bankD = bk[5 + hb]
                        Lt = Lt_r.get(); Mt = Mt_r.get()
                        A["Mt"].append(Mt)
                        for h in range(4):
                            P.I("pe", "matmul", out=bankD[:, h * 128:(h + 1) * 128], lhsT=arepm[:, hb * 4 + h, :], rhs=tri[:, TRI_LE if dirn == 0 else TRI_GE, :], start=True, stop=True)
                        P.I("act", "activation", out=Lt[:], in_=bankD[:], func=AF.Exp)
                        P.I("pool", "tensor_tensor", out=Mt[:], in0=Lt[:].rearrange("p (h q) -> p h q", h=4), in1=A["CBm"][dirn][:].unsq(1).bcast([128, 4, 128]), op=ALU.mult)
                return A

            def mainB(c, A, hinf):
                st = lat[c]
                yoff = [yoffF_r.get(), yoffB_r.get()]; yz = yz_r.get(); ssq = ssq_r.get(); sd = sd_r.get(); rstd = rstd_r.get(); yn = yn_r.get()
                P.I("pe", "matmul", out=bk[2][:], lhsT=st["CT"][:], rhs=hinf[:], start=True, stop=True)
                P.I("pe", "matmul", out=bk[3][:], lhsT=st["CT"][:], rhs=hin_b[c][:], start=True, stop=True)
                for dirn in range(2):
                    P.I("dve", "tensor_tensor", out=hv(yoff[dirn][:]), in0=hv(bk[2 + dirn][:]), in1=A["ecol"][:, dirn * 8:(dirn + 1) * 8].unsq(2).bcast([128, 8, 64]), op=ALU.mult)
                P.I("pe", "matmul", out=bk[4][:], lhsT=ident_b[:], rhs=A["xskip"][:], start=True, stop=False)
                P.I("pe", "matmul", out=bk[4][:], lhsT=ident_b[:], rhs=yoff[0][:], start=False, stop=False)
                P.I("pe", "matmul", out=bk[4][:], lhsT=ident_b[:], rhs=yoff[1][:], start=False, stop=False)
                for dirn in range(2):
                    for hb in range(2):
                        Mt = A["Mt"][dirn * 2 + hb]; xdt = A["xdt"][dirn]
                        for h in range(4):
                            hh = hb * 4 + h
                            P.I("pe", "matmul", out=bk[4][:, hh * 64:(hh + 1) * 64], lhsT=Mt[:, h, :], rhs=xdt[:, hh * 64:(hh + 1) * 64], start=False, stop=(dirn == 1 and hh == 7))
                P.I("dve", "tensor_tensor", out=yz[:], in0=bk[4][:], in1=A["zs"][:], op=ALU.mult)
                P.I("act", "activation", out=junk[:], in_=yz[:], func=AF.Square, accum_out=ssq[:])
                P.I("act", "activation", out=sd[:], in_=ssq[:], func=AF.Sqrt, scale=1.0 / 512.0, bias=eps6[:, 0:1])
                P.I("dve", "reciprocal", out=rstd[:], in_=sd[:])
                P.I("dve", "scalar_tensor_tensor", out=yn[:], in0=yz[:], scalar=rstd[:, 0:1], in1=ssdg[:, g * 512:(g + 1) * 512], op0=ALU.mult, op1=ALU.mult)
                for fc in range(4):
                    P.I("pe", "matmul", out=bk[7][:, fc * 128:(fc + 1) * 128], lhsT=yn[:, fc * 128:(fc + 1) * 128], rhs=ident_b[:], start=True, stop=True)
                P.I("act", "activation", out=ynT[:, :, (c % 4) * 128:(c % 4 + 1) * 128], in_=bk[7][:].rearrange("p (f t) -> p f t", f=4), func=AF.Identity)
                if c % 4 == 3:
                    for fc in range(4):
                        P.dma(cat[g * 4 + fc, :, (c // 4) * 512:(c // 4 + 1) * 512], ynT[:, fc, :])

            if b == 0 and g == 0:
                tap("xs0", lat[0]["xs"][:], [128, 512], BF16)
            A_next = mainA(0)
            for c in range(16):
                hf16 = hin_f.get()
                P.I("pool", "tensor_copy", out=hf16[:], in_=h_f[:])
                update(h_f, lat[c], 0, bk[7])
                A_cur = A_next
                if c + 1 < 16:
                    A_next = mainA(c + 1)
                mainB(c, A_cur, hf16)
            P.release(mp)

        for g in range(0 if "ssd" not in skip else 2, 2):
            ssd_pass(g)
            if b == 0 and g == 0:
                stop("ssd0")
        stop("ssd")

        ma = P.mark()
        cosT = P.sb("cosT", [128, T]); sinT = P.sb("sinT", [128, T])
        P.dma(cosT[:], D.c_cos); P.dma(sinT[:], D.c_sin)
        wsets = [dict(wq=P.sb(f"wq{i}", [128, 8, 256], BF16), wkd=P.sb(f"wkd{i}", [128, 8, 128], BF16), wv=P.sb(f"wv{i}", [128, 8, 64], BF16)) for i in range(2)]
        asets = [dict(KT=P.sb(f"KT{i}", [128, TC + T], BF16), VA=P.sb(f"VA{i}", [128, 18, 128], BF16), VB=P.sb(f"VB{i}", [128, 18, 128], BF16),
                      qT=P.sb(f"qT{i}", [128, 2, T], BF16)) for i in range(2)]
        for a_ in asets:
            P.I("pool", "memset", ap=a_["VA"][:], constant=1.0); P.I("pool", "memset", ap=a_["VB"][:], constant=1.0)
        nr_r = {n: Ring(P, n, 2, [128, 512], dt_) for n, dt_ in (("kgb", BF16), ("sqb", BF16), ("sdn", F32), ("rsn", F32), ("t1", F32), ("t2", F32))}
        PT = Ring(P, "PT", 3, [128, 1024], BF16); oTr = Ring(P, "oT", 2, [128, 2, 512], BF16); rec = P.sb("rec", [128, 512])

        def load_w(g):
            W = wsets[g % 2]
            for kc in range(8):
                st = stage.get()
                rows = slice(kc * 128, (kc + 1) * 128)
                P.dma(st[:, 0:256], D.w_in[rows, OFF_Q + g * 256:OFF_Q + (g + 1) * 256])
                P.dma(st[:, 256:320], D.w_in[rows, OFF_K + g * 64:OFF_K + (g + 1) * 64])
                P.dma(st[:, 320:384], D.w_in[rows, OFF_V + g * 64:OFF_V + (g + 1) * 64])
                P.I("pool", "tensor_copy", out=W["wq"][:, kc, :], in_=st[:, 0:256])
                P.I("pool", "tensor_copy", out=W["wkd"][:, kc, 0:64], in_=st[:, 256:320])
                P.I("pool", "tensor_copy", out=W["wkd"][:, kc, 64:128], in_=st[:, 256:320])
                P.I("pool", "tensor_copy", out=W["wv"][:, kc, :], in_=st[:, 320:384])

        def norm_rope(gvec, dst, n, tok0, bp, bs):
            kgb, sqb, sdn, rsn, t1, t2 = (nr_r[n_].get() for n_ in ("kgb", "sqb", "sdn", "rsn", "t1", "t2"))
            yield
            P.I("act", "activation", out=kgb[:, 0:n], in_=bp[:, 0:n], func=AF.Identity, scale=gvec[:, 0:1])
            P.I("act", "activation", out=sqb[:, 0:n], in_=bp[:, 0:n], func=AF.Square)
            yield
            P.I("pe", "matmul", out=bs[:, 0:n], lhsT=bones_b[:], rhs=sqb[:, 0:n], start=True, stop=True)
            if tok0 is not None:
                P.I("pe", "matmul", out=bp[:, 0:n], lhsT=prot_b[:], rhs=kgb[:, 0:n], start=True, stop=True)
                P.I("pool", "tensor_tensor", out=t1[:, 0:n], in0=kgb[:, 0:n], in1=cosT[:, tok0:tok0 + n], op=ALU.mult)
            yield
            P.I("act", "activation", out=sdn[:, 0:n], in_=bs[:, 0:n], func=AF.Sqrt, scale=1.0 / 64.0, bias=eps6[:, 0:1])
            if tok0 is not None:
                P.I("dve", "tensor_tensor", out=t2[:, 0:n], in0=bp[:, 0:n], in1=sinT[:, tok0:tok0 + n], op=ALU.mult)
            yield
            P.I("dve", "reciprocal", out=rsn[:, 0:n], in_=sdn[:, 0:n])
            if tok0 is None:
                yield
                P.I("dve", "tensor_tensor", out=dst, in0=kgb[:, 0:n], in1=rsn[:, 0:n], op=ALU.mult)
            else:
                P.I("pool", "tensor_tensor", out=t1[:, 0:n], in0=t1[:, 0:n], in1=t2[:, 0:n], op=ALU.add)
                yield
                P.I("dve", "tensor_tensor", out=dst, in0=t1[:, 0:n], in1=rsn[:, 0:n], op=ALU.mult)
            yield

        def qkv_units(g):
            W = wsets[g % 2]; S_ = asets[g % 2]
            ucnt = [0]

            def banks():
                ucnt[0] += 1
                return (bk[0], bk[1]) if ucnt[0] % 2 else (bk[2], bk[3])

            def vunit(hT, col0, t0, nt):
                _, bs = banks()
                for tt in range(nt):
                    for kc in range(8):
                        P.I("pe", "matmul", out=bs[:, tt * 64:(tt + 1) * 64], lhsT=hT[:, kc, col0 + tt * 128:col0 + (tt + 1) * 128], rhs=W["wv"][:, kc, :], start=(kc == 0), stop=(kc == 7))
                src = bs[:, 0:nt * 64].rearrange("p (t d) -> p t d", t=nt)
                yield
                P.I("act", "activation", out=S_["VA"][:, t0:t0 + nt, 0:64], in_=src, func=AF.Identity)
                P.I("dve", "tensor_copy", out=S_["VB"][:, t0:t0 + nt, 64:128], in_=src)

            bp, bs = banks()
            for kc in range(8):
                P.I("pe", "matmul", out=bp[:, 0:TC], lhsT=W["wkd"][:, kc, :], rhs=hcT[:, kc, 2:2 + TC], start=(kc == 0), stop=(kc == 7))
            yield from norm_rope(gk, S_["KT"][:, 0:TC], TC, None, bp, bs)
            yield from vunit(hcT, 2, 0, 2)
            yield
            for blk in range(4):
                tok0 = blk * 512
                bp, bs = banks()
                for kc in range(8):
                    P.I("pe", "matmul", out=bp[:], lhsT=W["wkd"][:, kc, :], rhs=hxT[:, kc, 2 + tok0:2 + tok0 + 512], start=(kc == 0), stop=(kc == 7))
                yield from norm_rope(gk, S_["KT"][:, TC + tok0:TC + tok0 + 512], 512, tok0, bp, bs)
                yield from vunit(hxT, 2 + tok0, 2 + blk * 4, 4)
                yield
                for j in range(2):
                    bp, bs = banks()
                    for kc in range(8):
                        P.I("pe", "matmul", out=bp[:], lhsT=W["wq"][:, kc, j * 128:(j + 1) * 128], rhs=hxT[:, kc, 2 + tok0:2 + tok0 + 512], start=(kc == 0), stop=(kc == 7))
                    yield from norm_rope(gq, S_["qT"][:, j, tok0:tok0 + 512], 512, tok0, bp, bs)

        sbanks = [bk[4], bk[5], bk[6], bk[7]]; obanks4 = [bk[0], bk[1], bk[2], bk[3]]
        its = [(qb, j, kt) for qb in range(4) for j in range(2) for kt in range(18)]

        def main_loop(g, filler):
            S_ = asets[g % 2]
            KT, VA, VB, qT = S_["KT"], S_["VA"], S_["VB"], S_["qT"]

            def s_mm(i):
                qb, j, kt = its[i]
                for half in range(2):
                    rows = slice(half * 64, half * 64 + 64)
                    P.I("pe", "matmul", out=spair[i % 2][:, half * 512:(half + 1) * 512], lhsT=KT[rows, kt * 128:(kt + 1) * 128], rhs=qT[rows, j, qb * 512:(qb + 1) * 512], start=True, stop=True)

            s_mm(0)
            oTb = None
            for i, (qb, j, kt) in enumerate(its):
                if i + 1 < len(its):
                    s_mm(i + 1)
                if j == 0 and kt == 0:
                    oTb = oTr.get()
                ptp = PT.get()
                obanks = obanks4[((i // 18) % 2) * 2:((i // 18) % 2) * 2 + 2]
                P.I("act", "activation", out=ptp[:], in_=spair[i % 2][:], func=AF.Exp, scale=0.125)
                for half in range(2):
                    P.I("pe", "matmul", out=obanks[half][:], lhsT=(VA if half == 0 else VB)[:, kt, :], rhs=ptp[:, half * 512:(half + 1) * 512], start=(kt == 0), stop=(kt == 17))
                if kt == 17:
                    P.I("dve", "reciprocal", out=rec[0:64, :], in_=obanks[0][64:128, :])
                    P.I("dve", "tensor_tensor", out=oTb[0:64, j, :], in0=obanks[0][0:64, :], in1=rec[0:64, :], op=ALU.mult)
                    P.I("dve", "reciprocal", out=rec[64:128, :], in_=obanks[1][0:64, :])
                    P.I("dve", "tensor_tensor", out=oTb[64:128, j, :], in0=obanks[1][64:128, :], in1=rec[64:128, :], op=ALU.mult)
                    if j == 1:
                        for jj in range(2):
                            P.dma(cat[8 + g * 2 + jj, :, qb * 512:(qb + 1) * 512], oTb[:, jj, :])
                if filler is not None:
                    next(filler, None)
            if filler is not None:
                for _ in filler:
                    pass

        load_w(0)
        for _ in qkv_units(0):
            pass
        load_w(1)
        for g in range(4):
            if g + 2 < 4:
                load_w(g + 2)
            main_loop(g, None)
            if g + 1 < 4:
                for _ in qkv_units(g + 1):
                    pass
        P.release(ma)
        stop("attn")
        P.release(mb)

        acc = [P.sb(f"acc{i}", [128, 1024]) for i in range(16)]
        hT = [P.sb(f"hT{i}", [128, 8, 512], BF16) for i in range(4)]
        comb = P.sb("comb", [128, 16, 16])
        sml = {n: P.sb("r_" + n, [128, w]) for n, w in (("lgt", 20), ("m", 1), ("negm", 1), ("oh", 4), ("eg", 4), ("se", 1), ("gval", 1), ("prod", 16),
                                                          ("esel", 4), ("m1", 1), ("oh1", 4), ("msk", 4), ("m2", 1), ("oh2", 4), ("d12", 1), ("e21", 1),
                                                          ("den", 1), ("w1", 1), ("w2", 1), ("tq", 4), ("wg", 4), ("ohg", 4), ("s1", 1), ("nm", 1),
                                                          ("ssq", 1), ("sd", 1), ("rstd", 1))}
        junkf = P.sb("junkf", [128, 1024], BF16)
        mo2 = P.mark()
        woH = [P.sb(f"woH{i}", [128, 1024], BF16) for i in range(8)]; catb_r = Ring(P, "catb", 2, [128, 8, 512], BF16)
        lng = P.sb("lng", [128, 1024]); lnb = P.sb("lnb", [128, 1024]); hf = P.sb("hf", [128, 8, 128])
        P.dma(lng[:], D.ln1_g.partition_broadcast(128)); P.dma(lnb[:], D.ln1_b.partition_broadcast(128))
        P.I("dve", "tensor_scalar", out=lnb[:], in0=lnb[:], scalar1=ALPHA, scalar2=None, op0=ALU.mult)

        def layer_norm_tile(a, g_t, b_t, post_scale):
            P.I("dve", "tensor_reduce", out=sml["s1"][:], in_=a[:], axis=AX.X, op=ALU.add)
            P.I("dve", "tensor_scalar", out=sml["nm"][:], in0=sml["s1"][:], scalar1=-1.0 / 1024.0, scalar2=None, op0=ALU.mult)
            P.I("act", "activation", out=a[:], in_=a[:], func=AF.Identity, bias=sml["nm"][:, 0:1])
            P.I("act", "activation", out=junkf[:], in_=a[:], func=AF.Square, accum_out=sml["ssq"][:])
            P.I("act", "activation", out=sml["sd"][:], in_=sml["ssq"][:], func=AF.Sqrt, scale=1.0 / 1024.0, bias=eps5[:, 0:1])
            P.I("dve", "reciprocal", out=sml["rstd"][:], in_=sml["sd"][:])
            if post_scale != 1.0:
                P.I("dve", "tensor_scalar", out=sml["rstd"][:], in0=sml["rstd"][:], scalar1=post_scale, scalar2=None, op0=ALU.mult)
            P.I("dve", "scalar_tensor_tensor", out=a[:], in0=a[:], scalar=sml["rstd"][:, 0:1], in1=g_t[:], op0=ALU.mult, op1=ALU.mult)
            P.I("dve", "tensor_tensor", out=a[:], in0=a[:], in1=b_t[:], op=ALU.add)

        def router(ti):
            S = sml
            g4 = lambda v: v.rearrange("p (g k) -> p g k", g=4)
            for c in range(8):
                P.I("pe", "matmul", out=bk[2][:, 0:20], lhsT=hf[:, c, :], rhs=wr[:, c, :], start=(c == 0), stop=(c == 7))
            P.I("dve", "tensor_tensor", out=S["lgt"][:], in0=bk[2][:, 0:20], in1=br[:], op=ALU.add)
            P.I("dve", "tensor_reduce", out=S["m"][:], in_=S["lgt"][:, 0:4], axis=AX.X, op=ALU.max)
            P.I("dve", "tensor_scalar", out=S["oh"][:], in0=S["lgt"][:, 0:4], scalar1=S["m"][:, 0:1], scalar2=None, op0=ALU.is_equal)
            P.I("dve", "tensor_scalar", out=S["negm"][:], in0=S["m"][:], scalar1=-1.0, scalar2=None, op0=ALU.mult)
            P.I("act", "activation", out=S["eg"][:], in_=S["lgt"][:, 0:4], func=AF.Exp, bias=S["negm"][:, 0:1], accum_out=S["se"][:])
            P.I("dve", "reciprocal", out=S["gval"][:], in_=S["se"][:])
            P.I("dve", "tensor_tensor", out=g4(S["prod"][:]), in0=g4(S["lgt"][:, 4:20]), in1=S["oh"][:].unsq(2).bcast([128, 4, 4]), op=ALU.mult)
            P.I("dve", "tensor_reduce", out=S["esel"][:], in_=S["prod"][:].rearrange("p (g k) -> p k g", g=4), axis=AX.X, op=ALU.add)
            P.I("dve", "tensor_reduce", out=S["m1"][:], in_=S["esel"][:], axis=AX.X, op=ALU.max)
            P.I("dve", "tensor_scalar", out=S["oh1"][:], in0=S["esel"][:], scalar1=S["m1"][:, 0:1], scalar2=None, op0=ALU.is_equal)
            P.I("dve", "scalar_tensor_tensor", out=S["msk"][:], in0=S["oh1"][:], scalar=-1e30, in1=S["esel"][:], op0=ALU.mult, op1=ALU.add)
            P.I("dve", "tensor_reduce", out=S["m2"][:], in_=S["msk"][:], axis=AX.X, op=ALU.max)
            P.I("dve", "tensor_scalar", out=S["oh2"][:], in0=S["msk"][:], scalar1=S["m2"][:, 0:1], scalar2=None, op0=ALU.is_equal)
            P.I("dve", "tensor_tensor", out=S["d12"][:], in0=S["m2"][:], in1=S["m1"][:], op=ALU.subtract)
            P.I("act", "activation", out=S["e21"][:], in_=S["d12"][:], func=AF.Exp)
            P.I("dve", "tensor_scalar", out=S["den"][:], in0=S["e21"][:], scalar1=1.0, scalar2=None, op0=ALU.add)
            P.I("dve", "reciprocal", out=S["w1"][:], in_=S["den"][:])
            P.I("dve", "tensor_tensor", out=S["w2"][:], in0=S["e21"][:], in1=S["w1"][:], op=ALU.mult)
            P.I("dve", "tensor_scalar", out=S["tq"][:], in0=S["oh1"][:], scalar1=S["w1"][:, 0:1], scalar2=None, op0=ALU.mult)
            P.I("dve", "scalar_tensor_tensor", out=S["wg"][:], in0=S["oh2"][:], scalar=S["w2"][:, 0:1], in1=S["tq"][:], op0=ALU.mult, op1=ALU.add)
            P.I("dve", "tensor_scalar", out=S["ohg"][:], in0=S["oh"][:], scalar1=S["gval"][:, 0:1], scalar2=None, op0=ALU.mult)
            P.I("dve", "tensor_tensor", out=g4(comb[:, ti, :]), in0=S["ohg"][:].unsq(2).bcast([128, 4, 4]), in1=S["wg"][:].unsq(1).bcast([128, 4, 4]), op=ALU.mult)

        def ln1_tile(ti):
            layer_norm_tile(acc[ti], lng, lnb, ALPHA)
            for c in range(8):
                P.I("pe", "transpose", out=bk[c // 4][:, (c % 4) * 128:(c % 4 + 1) * 128], in_=acc[ti][:, c * 128:(c + 1) * 128], identity=ident_f[:])
            for c in range(8):
                src = bk[c // 4][:, (c % 4) * 128:(c % 4 + 1) * 128]
                if c % 2 == 0:
                    P.I("act", "activation", out=hf[:, c, :], in_=src, func=AF.Identity, scale=modT[:, 32 + c, b:b + 1], bias=modT[:, 24 + c, b:b + 1])
                else:
                    P.I("dve", "tensor_scalar", out=hf[:, c, :], in0=src, scalar1=modT[:, 32 + c, b:b + 1], scalar2=modT[:, 24 + c, b:b + 1], op0=ALU.mult, op1=ALU.add)
            P.I("pool", "tensor_copy", out=hT[ti // 4][:, :, (ti % 4) * 128:(ti % 4 + 1) * 128], in_=hf[:])
            router(ti)

        for kh in range(2):
            for kc in range(8):
                st = stage.get()
                P.dma(st[:, 0:1024], D.w_out[(kh * 8 + kc) * 128:(kh * 8 + kc + 1) * 128, :])
                P.I("pool", "tensor_tensor", out=woH[kc][:], in0=st[:, 0:1024], in1=gA[:], op=ALU.mult)
            for blk in range(4):
                catb = catb_r.get()
                P.dma(catb[:], cat[kh * 8:(kh + 1) * 8, :, blk * 512:(blk + 1) * 512].rearrange("k p t -> p k t"))
                for tt in range(4):
                    ti = blk * 4 + tt
                    if kh == 0:
                        xt_ = xt.get()
                        P.dma(xt_[:], D.x[b, ti * 128:(ti + 1) * 128, :])
                    for half in range(2):
                        bank = bk[3 + (ti * 2 + half) % 5]
                        hs = slice(half * 512, (half + 1) * 512)
                        for kc in range(8):
                            P.I("pe", "matmul", out=bank[:], lhsT=catb[:, kc, tt * 128:(tt + 1) * 128], rhs=woH[kc][:, hs], start=(kc == 0), stop=(kc == 7))
                        if kh == 0:
                            P.I("dve", "scalar_tensor_tensor", out=acc[ti][:, hs], in0=xt_[:, hs], scalar=ALPHA, in1=bank[:], op0=ALU.mult, op1=ALU.add)
                        else:
                            P.I("dve", "tensor_tensor", out=acc[ti][:, hs], in0=bank[:], in1=acc[ti][:, hs], op=ALU.add)
                    if kh == 1:
                        ln1_tile(ti)
        if b == 0:
            tap("x1a0", acc[0][:], [128, 1024]); tap("comb", comb[:].rearrange("p t e -> p (t e)"), [128, 256])
            tap("hT0", hT[0][:].rearrange("p k t -> p (k t)"), [128, 4096], BF16)
        stop("ln1")
        P.release(mo2)

        lng2 = P.sb("lng2", [128, 1024]); P.dma(lng2[:], D.ln2_g.partition_broadcast(128))
        lnb2 = P.sb("lnb2", [128, 1024]); P.dma(lnb2[:], D.ln2_b.partition_broadcast(128))
        wgb = Ring(P, "wgb", 2, [128, 8, 256], BF16); wub = Ring(P, "wub", 2, [128, 8, 256], BF16); wdb = Ring(P, "wdb", 2, [128, 2, 1024], BF16)
        sgr = Ring(P, "sg", 2, [128, 2, 512], BF16); hidr = Ring(P, "hid", 2, [128, 2, 512], BF16)
        def load_expert(e):
            st = stage.get(); wg_ = wgb.get()
            P.dma(st[:].rearrange("p (k f) -> p k f", k=8), D.w_gate[e].rearrange("(k p) f -> p k f", p=128))
            P.I("pool", "tensor_copy", out=wg_[:].rearrange("p k f -> p (k f)"), in_=st[:])
            st = stage.get(); wu_ = wub.get()
            P.dma(st[:].rearrange("p (k f) -> p k f", k=8), D.w_up[e].rearrange("(k p) f -> p k f", p=128))
            P.I("pool", "tensor_copy", out=wu_[:].rearrange("p k f -> p (k f)"), in_=st[:])
            st = stage.get(); wd_ = wdb.get()
            P.dma(st[:].rearrange("p (k d) -> p k d", k=2), D.w_down[e].rearrange("(k p) d -> p k d", p=128))
            for k in range(2):
                P.I("pool", "tensor_tensor", out=wd_[:, k, :], in0=st[:, k * 1024:(k + 1) * 1024], in1=gF[:], op=ALU.mult)
            return wg_, wu_, wd_

        wts = {0: load_expert(0)}
        items = [(e, blk) for e in range(16) for blk in range(4)]
        hids = {}

        def gate_up(i):
            e, blk = items[i]
            if blk == 1 and e + 1 < 16:
                wts[e + 1] = load_expert(e + 1)
            wg_, wu_, _ = wts[e]
            for fc in range(2):
                for kc in range(8):
                    P.I("pe", "matmul", out=bk[2 * fc][:], lhsT=wg_[:, kc, fc * 128:(fc + 1) * 128], rhs=hT[blk][:, kc, :], start=(kc == 0), stop=(kc == 7))
                for kc in range(8):
                    P.I("pe", "matmul", out=bk[2 * fc + 1][:], lhsT=wu_[:, kc, fc * 128:(fc + 1) * 128], rhs=hT[blk][:, kc, :], start=(kc == 0), stop=(kc == 7))
            sg = sgr.get(); hid = hidr.get()
            for fc in range(2):
                P.I("act", "activation", out=sg[:, fc, :], in_=bk[2 * fc][:], func=AF.Silu)
                P.I("dve", "tensor_tensor", out=hid[:, fc, :], in0=bk[2 * fc + 1][:], in1=sg[:, fc, :], op=ALU.mult)
            hids[i] = hid

        def down(i):
            e, blk = items[i]
            hid = hids.pop(i); wd_ = wts[e][2]
            for tt in range(4):
                ti = blk * 4 + tt
                for half in range(2):
                    bank = bk[4 + (tt * 2 + half) % 4]
                    hs = slice(half * 512, (half + 1) * 512)
                    for fc in range(2):
                        P.I("pe", "matmul", out=bank[:], lhsT=hid[:, fc, tt * 128:(tt + 1) * 128], rhs=wd_[:, fc, hs], start=(fc == 0), stop=(fc == 1))
                    P.I("dve", "scalar_tensor_tensor", out=acc[ti][:, hs], in0=bank[:], scalar=comb[:, ti, e:e + 1], in1=acc[ti][:, hs], op0=ALU.mult, op1=ALU.add)

        gate_up(0)
        for i in range(len(items)):
            if i + 1 < len(items):
                gate_up(i + 1)
            down(i)
        if b == 0:
            tap("pre2", acc[0][:], [128, 1024])
        for ti in range(16):
            layer_norm_tile(acc[ti], lng2, lnb2, 1.0)
            P.dma(ybuf[b, ti * 128:(ti + 1) * 128, :], acc[ti][:])
        stop("moe")
        P.release(mb)

    P.barrier()
    return nc, P


def finish(nc, P):
    P.barrier()
    P.build()
    return nc


def _const_tables():
    i = np.arange(128)
    le = (i[:, None] <= i[None, :]).astype(np.float32)
    ge = (i[:, None] >= i[None, :]).astype(np.float32)
    gt = (i[:, None] > i[None, :]).astype(np.float32)
    lt = (i[:, None] < i[None, :]).astype(np.float32)
    tri = np.stack([le, ge, gt, lt, np.ones((128, 128), np.float32)])
    prot = np.zeros((128, 128), np.float32)
    for j in range(64):
        prot[2 * j + 1, 2 * j] = -1.0
        prot[2 * j, 2 * j + 1] = 1.0
    bones = (i[:, None] // 64 == i[None, :] // 64).astype(np.float32)
    t = np.arange(T)
    rows = (t // 64).astype(np.float32)
    cols = (t % 64).astype(np.float32)
    inv_freq = np.power(np.float32(10000.0), -np.arange(0, 32, 2, dtype=np.float32) / np.float32(32)).astype(np.float32)
    ang = np.concatenate([rows[:, None] * inv_freq, cols[:, None] * inv_freq], -1).astype(np.float32)
    pair = (i % 64) // 2
    return {"c_ident": np.eye(128, dtype=np.float32), "c_tri": tri, "c_prot": prot, "c_bones": bones,
            "c_cos": np.ascontiguousarray(np.cos(ang)[:, pair].T.astype(np.float32)),
            "c_sin": np.ascontiguousarray(np.sin(ang)[:, pair].T.astype(np.float32))}


def _core_inputs(d, core, consts):
    b0 = core * NB
    f = lambda a: np.ascontiguousarray(np.asarray(a, dtype=np.float32))
    m = {"x": d["x"][b0:b0 + NB], "ctx": d["ctx"][b0:b0 + NB],
         "crow": np.concatenate([np.asarray(d["c"])[b0:b0 + NB], np.asarray(d["c_ctx"])[None]], 0),
         "w_mod": d["w_mod"][0], "b_mod": d["b_mod"], "w_in": d["w_in"][0], "conv_w": d["conv_w"][0], "conv_b": d["conv_b"],
         "dt_bias": np.asarray(d["dt_bias"]).reshape(1, 32), "a_log": np.asarray(d["a_log"]).reshape(1, 32), "d_skip": d["d_skip"],
         "ssd_norm_g": d["ssd_norm_g"], "q_norm_g": np.asarray(d["q_norm_g"]).reshape(64, 1), "k_norm_g": np.asarray(d["k_norm_g"]).reshape(64, 1),
         "w_out": d["w_out"][0], "ln1_g": d["ln1_g"], "ln1_b": d["ln1_b"], "ln2_g": d["ln2_g"], "ln2_b": d["ln2_b"],
         "w_r": np.concatenate([np.asarray(d["w_rg"])[0], np.asarray(d["w_re"])[0]], 1),
         "b_r": np.concatenate([np.asarray(d["b_rg"]), np.asarray(d["b_re"])], 1),
         "w_gate": d["w_gate"][0], "w_up": d["w_up"][0], "w_down": d["w_down"][0]}
    m.update(consts)
    return {k: f(v) for k, v in m.items()}


def kernel(**inputs):
    n_cores = 8
    nc, P = build_program()
    finish(nc, P)
    consts = _const_tables()
    shared = None
    in_maps = []
    for core in range(n_cores):
        ci = _core_inputs(inputs, core, consts)
        if shared is None:
            shared = ci
        else:
            for k in ci:
                if k not in ("x", "ctx", "crow"):
                    ci[k] = shared[k]
        in_maps.append({k: ci[k] for k in used_inputs})
    res = run_bass_kernel_spmd(nc, in_maps, core_ids=list(range(n_cores)))
    return np.concatenate([np.asarray(r["y"]) for r in res.results], axis=0).astype(np.float32)
```

```python
import numpy as np
import concourse.bass as bass
import concourse.mybir as mybir
from concourse.bass_utils import run_bass_kernel_spmd

F32 = mybir.dt.float32
BF16 = mybir.dt.bfloat16
AF = mybir.ActivationFunctionType
ALU = mybir.AluOpType
AX = mybir.AxisListType

SB_BASE = 16512
SB_END = 229376


class Buf:
    def __init__(self, t, name):
        self.t = t
        self.name = name
        self.w = None
        self.r = {}
        self.dsem = None
        self.excl = False

    def __getitem__(self, k):
        return View(self, self.t[k])


class View:
    def __init__(self, buf, ap):
        self.buf = buf
        self.ap = ap

    def __getitem__(self, k):
        return View(self.buf, self.ap[k])

    def rearrange(self, s, **kw):
        return View(self.buf, self.ap.rearrange(s, **kw))

    def bcast(self, shape):
        return View(self.buf, self.ap.broadcast_to(list(shape)))

    def unsq(self, axis):
        return View(self.buf, self.ap.unsqueeze(axis))

    def bitcast(self, dt):
        return View(self.buf, self.ap.bitcast(dt))

    @property
    def shape(self):
        return tuple(self.ap.shape)


WRITE_KEYS = ("out", "accum_out", "ap")


class Prog:
    ENGS = ("pe", "act", "dve", "pool", "sp")

    def __init__(self, nc):
        self.nc = nc
        self.sems = {}
        self.cnt = {}
        self.seen = {e: {} for e in self.ENGS}
        self.streams = {e: [] for e in self.ENGS}
        self.top = SB_BASE
        self.ninst = 0
        for e in self.ENGS:
            self._mksem(e)

    def _mksem(self, name):
        self.sems[name] = self.nc.alloc_semaphore(name)
        self.cnt[name] = 0

    def sb(self, name, shape, dtype=F32):
        nbytes = int(np.prod(shape[1:])) * (4 if dtype == F32 else 2)
        nbytes = (nbytes + 63) // 64 * 64
        off = self.top
        assert off + nbytes <= SB_END, f"SBUF overflow allocating {name}: {off + nbytes - SB_BASE}"
        self.top += nbytes
        self.maxtop = max(getattr(self, 'maxtop', 0), self.top)
        uname = f"{name}_{off}"
        self.nalloc = getattr(self, "nalloc", 0) + 1
        return Buf(self.nc.alloc_sbuf_tensor_at(f"{uname}_{self.nalloc}", list(shape), dtype, offset=off), uname)

    def mark(self):
        return self.top

    def release(self, mark):
        self.barrier()
        self.top = mark

    def ps(self, name, shape, dtype=F32):
        b = Buf(self.nc.alloc_psum_tensor(name, list(shape), dtype), name)
        b.excl = True
        return b

    def dram(self, name, shape, dtype, kind="Internal"):
        return Buf(self.nc.dram_tensor(name, list(shape), dtype, kind=kind).ap(), name)

    @staticmethod
    def _add(deps, tok):
        if tok is None:
            return
        s, v = tok
        if deps.get(s, 0) < v:
            deps[s] = v

    def _deps(self, reads, writes):
        deps = {}
        for b in reads:
            self._add(deps, b.w)
        for b in writes:
            self._add(deps, b.w)
            for s, v in b.r.items():
                self._add(deps, (s, v))
        return deps

    def _emit_waits(self, e, deps, skip_self):
        seen = self.seen[e]
        for s, v in deps.items():
            if s == e and skip_self:
                continue
            if seen.get(s, 0) >= v:
                continue
            seen[s] = v
            sem = self.sems[s]
            self.streams[e].append(lambda eng, sem=sem, v=v: eng.wait_ge(sem, v))

    def _mark(self, tok, reads, writes):
        s, v = tok
        for b in reads:
            if b.r.get(s, 0) < v:
                b.r[s] = v
        for b in writes:
            b.w = tok
            b.r = {}

    @staticmethod
    def _split(kw):
        reads, writes, real = [], [], {}
        for k, a in kw.items():
            if isinstance(a, View):
                (writes if (k in WRITE_KEYS or a.buf.excl) else reads).append(a.buf)
                real[k] = a.ap
            else:
                real[k] = a
        return reads, writes, real

    def I(self, e, method, **kw):
        reads, writes, real = self._split(kw)
        if method == "matmul" and kw.get("start") is False:
            reads = reads + writes
        deps = self._deps(reads, writes)
        self._emit_waits(e, deps, skip_self=(e == "pe"))
        self.cnt[e] += 1
        sem = self.sems[e]
        self.streams[e].append(lambda eng, m=method, real=real, sem=sem: getattr(eng, m)(**real).then_inc(sem, 1))
        self._mark((e, self.cnt[e]), reads, writes)
        self.ninst += 1

    def dma(self, out, in_, q="sp", **kw):
        reads, writes = [], []
        if isinstance(in_, View):
            reads.append(in_.buf)
            in_ = in_.ap
        if isinstance(out, View):
            writes.append(out.buf)
            out = out.ap
        deps = self._deps(reads, writes)
        self._emit_waits(q, deps, skip_self=True)
        tb = writes[0] if writes else reads[0]
        if tb.dsem is None:
            tb.dsem = "d_" + tb.name
            if tb.dsem not in self.sems:
                self._mksem(tb.dsem)
        s = tb.dsem
        self.cnt[s] += 16
        sem = self.sems[s]
        self.streams[q].append(lambda eng, out=out, in_=in_, sem=sem, kw=kw: eng.dma_start(out=out, in_=in_, **kw).then_inc(sem, 16))
        self._mark((s, self.cnt[s]), reads, writes)
        self.ninst += 1

    def barrier(self):
        for e in self.ENGS:
            deps = {s: v for s, v in self.cnt.items() if v > 0}
            self._emit_waits(e, deps, skip_self=(e == "pe"))

    def build(self):
        with self.nc.Block() as block:
            def mk(e):
                def run(eng):
                    for f in self.streams[e]:
                        f(eng)
                return run
            block.tensor(mk("pe"))
            block.scalar(mk("act"))
            block.vector(mk("dve"))
            block.gpsimd(mk("pool"))
            block.sync(mk("sp"))


class Ring:
    def __init__(self, P, name, n, shape, dtype=F32):
        self.bufs = [P.sb(f"{name}{i}", shape, dtype) for i in range(n)]
        self.i = 0

    def get(self):
        b = self.bufs[self.i % len(self.bufs)]
        self.i += 1
        return b


OFF_Z, OFF_Q, OFF_XBC, OFF_DT, OFF_K, OFF_V, D_IN = 0, 1024, 2048, 3584, 3616, 3872, 4128
T = 2048
TC = 256
ALPHA = 2.0 ** 0.25
NB = 2


class StopBuild(Exception):
    pass


_last = [None, None]
used_inputs = []


def build_program(dbg=(), stop_after=None, nb=NB, skip=()):
    nc = bass.Bass("TRN2", target_bir_lowering=False)
    P = Prog(nc)
    _last[:] = [nc, P]

    in_shapes = {
        "x": [NB, T, 1024], "ctx": [NB, TC, 1024], "crow": [3, 1024], "w_mod": [1024, 6144], "b_mod": [1, 6144],
        "w_in": [1024, D_IN], "conv_w": [5, 1536], "conv_b": [1, 1536], "dt_bias": [1, 32], "a_log": [1, 32], "d_skip": [1, 16],
        "ssd_norm_g": [1, 1024], "q_norm_g": [64, 1], "k_norm_g": [64, 1], "w_out": [2048, 1024],
        "ln1_g": [1, 1024], "ln1_b": [1, 1024], "ln2_g": [1, 1024], "ln2_b": [1, 1024], "w_r": [1024, 20], "b_r": [1, 20],
        "w_gate": [16, 1024, 256], "w_up": [16, 1024, 256], "w_down": [16, 256, 1024],
        "c_ident": [128, 128], "c_tri": [5, 128, 128], "c_prot": [128, 128], "c_bones": [128, 128], "c_cos": [128, T], "c_sin": [128, T],
    }
    in_aps = {}
    used_inputs.clear()

    class _D:
        def __getattr__(self, name):
            if name not in in_aps:
                in_aps[name] = nc.dram_tensor(name, list(in_shapes[name]), F32, kind="ExternalInput").ap()
                used_inputs.append(name)
            return in_aps[name]
    D = _D()
    y_d = nc.dram_tensor("y", [NB, T, 1024], F32, kind="ExternalOutput").ap()
    cat = P.dram("cat", [16, 128, T], BF16)
    ybuf = Buf(y_d, "ybuf")

    def tap(name, view, shape, dt=F32):
        if name in dbg:
            o = nc.dram_tensor("dbg_" + name, list(shape), dt, kind="ExternalOutput").ap()
            P.dma(o, view)

    def stop(name):
        if stop_after == name:
            raise StopBuild()

    bk = [P.ps(f"bank{i}", [128, 512]) for i in range(4)]
    spair = []
    for i in range(2):
        pt_ = nc.alloc_psum_tensor(f"pp{i}", [128, 1024], F32)
        for vw in (pt_[:, 0:512], pt_[:, 512:1024]):
            b_ = Buf(vw, f"bank{len(bk)}"); b_.excl = True; bk.append(b_)
        b_ = Buf(pt_[:, :], f"spair{i}"); b_.excl = True; spair.append(b_)

    ident_f = P.sb("ident_f", [128, 128]); ident_b = P.sb("ident_b", [128, 128], BF16)
    tri = P.sb("tri", [128, 5, 128])
    TRI_LE, TRI_GE, TRI_GT, TRI_LT, TRI_ONE = range(5)
    prot_b = P.sb("prot_b", [128, 128], BF16); bones_b = P.sb("bones_b", [128, 128], BF16)
    convw = P.sb("convw", [128, 12, 5]); convb = P.sb("convb", [128, 12])
    gq = P.sb("gq", [128, 1]); gk = P.sb("gk", [128, 1])
    ssdg = P.sb("ssdg", [128, 1024]); dskip = P.sb("dskip", [128, 16]); dtb = P.sb("dtb", [128, 32]); Aneg = P.sb("Aneg", [128, 32])
    br = P.sb("br", [128, 20]); wr = P.sb("wr", [128, 8, 20])
    modT = P.sb("modT", [128, 48, 3])
    gA = P.sb("gA", [128, 1024]); gF = P.sb("gF", [128, 1024])
    eps5 = P.sb("eps5", [128, 1]); eps6 = P.sb("eps6", [128, 1])
    stage = Ring(P, "stage", 2, [128, 2048])
    xt = Ring(P, "xt", 4, [128, 1024])

    P.dma(ident_f[:], D.c_ident)
    P.dma(tri[:], D.c_tri.rearrange("k p q -> p k q"))
    P.I("pool", "tensor_copy", out=ident_b[:], in_=ident_f[:])
    s0 = stage.get()
    P.dma(s0[:, 0:128], D.c_prot); P.dma(s0[:, 128:256], D.c_bones)
    P.I("pool", "tensor_copy", out=prot_b[:], in_=s0[:, 0:128])
    P.I("pool", "tensor_copy", out=bones_b[:], in_=s0[:, 128:256])
    for c in range(12):
        P.dma(convw[:, c, :], D.conv_w[:, c * 128:(c + 1) * 128].rearrange("k p -> p k"), allow_slow_non_contiguous=True)
        P.dma(convb[:, c:c + 1], D.conv_b[:, c * 128:(c + 1) * 128].rearrange("k p -> p k"), allow_slow_non_contiguous=True)
    for h in range(2):
        P.dma(gq[h * 64:(h + 1) * 64, :], D.q_norm_g); P.dma(gk[h * 64:(h + 1) * 64, :], D.k_norm_g)
    P.dma(ssdg[:], D.ssd_norm_g.partition_broadcast(128)); P.dma(dskip[:], D.d_skip.partition_broadcast(128))
    P.dma(dtb[:], D.dt_bias.partition_broadcast(128)); P.dma(Aneg[:], D.a_log.partition_broadcast(128))
    P.dma(br[:], D.b_r.partition_broadcast(128))
    P.dma(wr[:], D.w_r.rearrange("(k p) n -> p k n", p=128))
    P.I("act", "activation", out=Aneg[:], in_=Aneg[:], func=AF.Exp)
    P.I("dve", "tensor_scalar", out=Aneg[:], in0=Aneg[:], scalar1=-1.0, scalar2=None, op0=ALU.mult)
    P.I("dve", "memset", ap=eps5[:], constant=1e-5); P.I("dve", "memset", ap=eps6[:], constant=1e-6)

    m0 = P.mark()
    crow = P.sb("crow", [3, 1024]); scT = P.sb("scT", [128, 8, 3]); bmod3 = P.sb("bmod3", [3, 6144]); modrow = P.sb("modrow", [3, 6144])
    P.dma(crow[:], D.crow)
    P.dma(bmod3[:], D.b_mod.partition_broadcast(3))
    P.I("act", "activation", out=crow[:], in_=crow[:], func=AF.Silu)
    for kc in range(8):
        P.I("pe", "transpose", out=bk[0][:, kc * 3:(kc + 1) * 3], in_=crow[0:3, kc * 128:(kc + 1) * 128], identity=ident_f[0:3, 0:3])
    P.I("dve", "tensor_copy", out=scT[:].rearrange("p k r -> p (k r)"), in_=bk[0][:, 0:24])
    wm_r = Ring(P, "wmst", 6, [128, 2048])
    for third in range(3):
        for kc in range(8):
            st = wm_r.get()
            P.dma(st[:], D.w_mod[kc * 128:(kc + 1) * 128, third * 2048:(third + 1) * 2048])
            for j in range(4):
                P.I("pe", "matmul", out=bk[1 + j][0:3, :], lhsT=scT[:, kc, :], rhs=st[:, j * 512:(j + 1) * 512], start=(kc == 0), stop=(kc == 7))
        for j in range(4):
            c0 = third * 2048 + j * 512
            P.I("dve", "tensor_tensor", out=modrow[:, c0:c0 + 512], in0=bk[1 + j][0:3, :], in1=bmod3[:, c0:c0 + 512], op=ALU.add)
    for j in range(48):
        P.I("pe", "transpose", out=bk[0][:, j * 3:(j + 1) * 3], in_=modrow[0:3, j * 128:(j + 1) * 128], identity=ident_f[0:3, 0:3])
    P.I("dve", "tensor_copy", out=modT[:].rearrange("p k r -> p (k r)"), in_=bk[0][:, 0:144])
    P.I("dve", "tensor_scalar", out=modT[:, 8:16, :], in0=modT[:, 8:16, :], scalar1=1.0, scalar2=None, op0=ALU.add)
    P.I("dve", "tensor_scalar", out=modT[:, 32:40, :], in0=modT[:, 32:40, :], scalar1=1.0, scalar2=1.0 / ALPHA, op0=ALU.add, op1=ALU.mult)
    tap("modT", modT[:].rearrange("p k r -> p (k r)"), [128, 144])
    P.release(m0)
    stop("mod")

    def bcast_row(dst, sec, row):
        rep = stage.get()
        for c in range(8):
            P.I("dve", "tensor_copy", out=rep[:, c * 128:(c + 1) * 128], in_=modT[:, sec * 8 + c, row:row + 1].bcast([128, 128]))
        for c in range(8):
            P.I("pe", "matmul", out=bk[c // 4][:, (c % 4) * 128:(c % 4 + 1) * 128], lhsT=rep[:, c * 128:(c + 1) * 128], rhs=ident_f[:], start=True, stop=True)
        for hh in range(2):
            P.I("act", "activation", out=dst[:, hh * 512:(hh + 1) * 512], in_=bk[hh][:], func=AF.Identity)

    for b in range(nb):
        mb = P.mark()
        bcast_row(gA, 2, b)
        bcast_row(gF, 5, b)
        tap(f"gA{b}", gA[:], [128, 1024])
        hxT = P.sb("hxT", [128, 8, T + 4], BF16)
        hcT = P.sb("hcT", [128, 8, TC + 4], BF16)
        P.I("pool", "memset", ap=hxT[:, :, 0:2], constant=0.0); P.I("pool", "memset", ap=hxT[:, :, T + 2:T + 4], constant=0.0)
        P.I("pool", "memset", ap=hcT[:, :, 0:2], constant=0.0); P.I("pool", "memset", ap=hcT[:, :, TC + 2:TC + 4], constant=0.0)

        mhx = P.mark()
        xr = Ring(P, "xr", 8, [128, 1024])

        def build_hT(dstT, src_d, ntile, row):
            for blk in range((ntile + 3) // 4):
                nt = min(4, ntile - blk * 4)
                tiles = []
                for tt in range(nt):
                    t_ = xr.get()
                    P.dma(t_[:], src_d[(blk * 4 + tt) * 128:(blk * 4 + tt + 1) * 128, :])
                    tiles.append(t_)
                for c in range(8):
                    bank = bk[c % 4]
                    for tt in range(nt):
                        P.I("pe", "transpose", out=bank[:, tt * 128:(tt + 1) * 128], in_=tiles[tt][:, c * 128:(c + 1) * 128], identity=ident_f[:])
                    dst = dstT[:, c, 2 + blk * 512:2 + blk * 512 + nt * 128]
                    if c % 2 == 0:
                        P.I("act", "activation", out=dst, in_=bank[:, 0:nt * 128], func=AF.Identity, scale=modT[:, 8 + c, row:row + 1], bias=modT[:, c, row:row + 1])
                    else:
                        P.I("dve", "tensor_scalar", out=dst, in0=bank[:, 0:nt * 128], scalar1=modT[:, 8 + c, row:row + 1], scalar2=modT[:, c, row:row + 1], op0=ALU.mult, op1=ALU.add)

        build_hT(hcT, D.ctx[b], 2, 2)
        build_hT(hxT, D.x[b], 16, b)
        P.release(mhx)
        if b == 0:
            tap("hxT", hxT[:].rearrange("p k t -> p (k t)"), [128, 8 * (T + 4)], BF16)
            tap("hcT", hcT[:].rearrange("p k t -> p (k t)"), [128, 8 * (TC + 4)], BF16)
        stop("hx")

        def load_cols(dst_w, col_specs):
            for kc in range(8):
                st = stage.get()
                o = 0
                for (c0, n) in col_specs:
                    P.dma(st[:, o:o + n], D.w_in[kc * 128:(kc + 1) * 128, c0:c0 + n])
                    o += n
                P.I("pool", "tensor_copy", out=dst_w[kc][:, 0:o], in_=st[:, 0:o])

        hv = lambda v: v.rearrange("p (h j) -> p h j", h=8)

        def ssd_pass(g):
            mp = P.mark()
            wZ = [P.sb(f"wZ{i}", [128, 512], BF16) for i in range(8)]
            cidx = [g * 4, g * 4 + 1, g * 4 + 2, g * 4 + 3, 8 + g, 10 + g]

            dtA = P.sb("dtA", [128, 18, 16]); aA = P.sb("aA", [128, 18, 16])
            ewA = [P.sb("ewF", [128, 18, 16]), P.sb("ewB", [128, 18, 16])]

            def mkstore(tag, n, i0):
                return [dict(xs=P.sb(f"xs{tag}{i}", [128, 512], BF16), Bt=P.sb(f"Bt{tag}{i}", [128, 128], BF16),
                             BT=P.sb(f"BT{tag}{i}", [128, 128], BF16), CT=P.sb(f"CT{tag}{i}", [128, 128], BF16),
                             dt=dtA[:, i0 + i, :], a=aA[:, i0 + i, :], idx=i0 + i) for i in range(n)]
            lat = mkstore("L", 16, 0)
            hin_b = [P.sb(f"hinb{i}", [128, 512], BF16) for i in range(16)]
            hin_f = Ring(P, "hinf", 3, [128, 512], BF16)
            h_f = P.sb("h_f", [128, 512]); h_b = P.sb("h_b", [128, 512])
            sm_r = {n: Ring(P, "sm_" + n, 2, [128, 16]) for n in ("sc", "ecol")}
            xw_r = Ring(P, "xw", 2, [128, 512], BF16)

            def update(h, st, dirn, bank_s):
                sc = sm_r["sc"].get(); xw = xw_r.get()
                ew = ewA[dirn][:, st["idx"], :]
                P.I("dve", "tensor_tensor", out=sc[:, 0:8], in0=st["dt"][:, dirn * 8:(dirn + 1) * 8], in1=ew[:, 0:8], op=ALU.mult)
                P.I("dve", "tensor_tensor", out=hv(xw[:]), in0=hv(st["xs"][:]), in1=sc[:, 0:8].unsq(2).bcast([128, 8, 64]), op=ALU.mult)
                P.I("pe", "matmul", out=bank_s[:], lhsT=st["Bt"][:], rhs=xw[:], start=True, stop=True)
                P.I("pool", "tensor_tensor", out=hv(h[:]), in0=hv(h[:]), in1=ew[:, 8:16].unsq(2).bcast([128, 8, 64]), op=ALU.mult)
                P.I("dve", "tensor_tensor", out=h[:], in0=bank_s[:], in1=h[:], op=ALU.add)

            m1 = P.mark()
            wS = [P.sb(f"wS{i}", [128, 784], BF16) for i in range(8)]
            load_cols(wS, [(OFF_XBC + g * 512, 512), (OFF_XBC + 1024 + g * 128, 128), (OFF_XBC + 1280 + g * 128, 128),
                           (OFF_DT + g * 8, 8), (OFF_DT + 16 + g * 8, 8)])
            load_cols(wZ, [(OFF_Z + g * 512, 512)])
            cst = mkstore("C", 2, 16)
            rawS_r = Ring(P, "rawS", 2, [128, 6, 132], BF16); xcT_r = Ring(P, "xcT", 2, [128, 4, 128], BF16)
            dg = P.sb("dg", [128, 6, 5, 128], BF16)
            for r in range(6):
                for k in range(5):
                    P.I("dve", "tensor_scalar", out=dg[:, r, k, :], in0=ident_f[:], scalar1=convw[:, cidx[r], k:k + 1], scalar2=None, op0=ALU.mult)

            def prep(hT, c, st, need_C):
                rawS = rawS_r.get(); xcT = xcT_r.get()
                tok0 = c * 128
                nr = 6 if need_C else 5
                for r in range(nr):
                    bank = bk[r // 3]
                    o = (r % 3) * 132
                    for kc in range(8):
                        P.I("pe", "matmul", out=bank[:, o:o + 132], lhsT=wS[kc][:, r * 128:(r + 1) * 128], rhs=hT[:, kc, tok0:tok0 + 132], start=(kc == 0), stop=(kc == 7))
                P.I("act", "activation", out=rawS[:, 0:3, :].rearrange("p r t -> p (r t)"), in_=bk[0][:, 0:396], func=AF.Identity)
                P.I("act", "activation", out=rawS[:, 3:nr, :].rearrange("p r t -> p (r t)"), in_=bk[1][:, 0:(nr - 3) * 132], func=AF.Identity)
                for r in range(nr):
                    cb = bk[2][:, r * 128:(r + 1) * 128] if r < 4 else bk[3][:, (r - 4) * 128:(r - 3) * 128]
                    for k in range(5):
                        P.I("pe", "matmul", out=cb, lhsT=dg[:, r, k, :], rhs=rawS[:, r, k:k + 128], start=(k == 0), stop=(k == 4))
                for r in range(nr):
                    cb = bk[2][:, r * 128:(r + 1) * 128] if r < 4 else bk[3][:, (r - 4) * 128:(r - 3) * 128]
                    dst = xcT[:, r, :] if r < 4 else (st["BT"][:] if r == 4 else st["CT"][:])
                    P.I("act", "activation", out=dst, in_=cb, func=AF.Silu, bias=convb[:, cidx[r]:cidx[r] + 1])
                for r in range(4):
                    P.I("pe", "matmul", out=bk[6][:, r * 128:(r + 1) * 128], lhsT=xcT[:, r, :], rhs=ident_b[:], start=True, stop=True)
                P.I("pe", "matmul", out=bk[7][:, 0:128], lhsT=st["BT"][:], rhs=ident_b[:], start=True, stop=True)
                P.I("act", "activation", out=st["xs"][:], in_=bk[6][:], func=AF.Identity)
                P.I("dve", "tensor_copy", out=st["Bt"][:], in_=bk[7][:, 0:128])

            big = {n: P.sb("big_" + n, [128, 18, 16]) for n in ("u", "nu", "na", "e", "l")}
            for idx in range(18):
                hT_, tok0_ = (hxT, idx * 128) if idx < 16 else (hcT, (idx - 16) * 128)
                for kc in range(8):
                    P.I("pe", "matmul", out=bk[3][:, idx * 16:(idx + 1) * 16], lhsT=hT_[:, kc, 2 + tok0_:2 + tok0_ + 128], rhs=wS[kc][:, 768:784], start=(kc == 0), stop=(kc == 7))
            d4 = lambda v: v.rearrange("p i (d h) -> p i d h", d=2)
            fl = lambda v: v.rearrange("p i x -> p (i x)")
            dvg = lambda v: v.rearrange("p (d h) -> p d h", d=2)[:, :, g * 8:(g + 1) * 8].unsq(1).bcast([128, 18, 2, 8])
            P.I("dve", "tensor_tensor", out=d4(big["u"][:]), in0=d4(bk[3][:, 0:288].rearrange("p (i x) -> p i x", i=18)), in1=dvg(dtb[:]), op=ALU.add)
            P.I("dve", "tensor_scalar", out=fl(big["nu"][:]), in0=fl(big["u"][:]), scalar1=-1.0, scalar2=None, op0=ALU.mult)
            P.I("dve", "tensor_tensor", out=fl(big["na"][:]), in0=fl(big["u"][:]), in1=fl(big["nu"][:]), op=ALU.min)
            P.I("act", "activation", out=fl(big["e"][:]), in_=fl(big["na"][:]), func=AF.Exp)
            P.I("act", "activation", out=fl(big["l"][:]), in_=fl(big["e"][:]), func=AF.Ln, bias=1.0, scale=1.0)
            P.I("dve", "scalar_tensor_tensor", out=fl(dtA[:]), in0=fl(big["u"][:]), scalar=0.0, in1=fl(big["l"][:]), op0=ALU.max, op1=ALU.add)
            P.I("dve", "tensor_tensor", out=d4(aA[:]), in0=d4(dtA[:]), in1=dvg(Aneg[:]), op=ALU.mult)
            for dirn in range(2):
                for idx in range(18):
                    a_d = aA[:, idx, dirn * 8:(dirn + 1) * 8]
                    P.I("pe", "matmul", out=bk[4 + dirn][:, idx * 16:idx * 16 + 8], lhsT=tri[:, TRI_GT if dirn == 0 else TRI_LT, :], rhs=a_d, start=True, stop=True)
                    P.I("pe", "matmul", out=bk[4 + dirn][:, idx * 16 + 8:idx * 16 + 16], lhsT=tri[:, TRI_ONE, :], rhs=a_d, start=True, stop=True)
                P.I("act", "activation", out=fl(ewA[dirn][:]), in_=bk[4 + dirn][:, 0:288], func=AF.Exp)

            for cc in range(2):
                prep(hcT, cc, cst[cc], False)
            P.I("pool", "memset", ap=h_f[:], constant=0.0); P.I("pool", "memset", ap=h_b[:], constant=0.0)
            prep(hxT, 15, lat[15], True)
            update(h_f, cst[0], 0, bk[5]); update(h_f, cst[1], 0, bk[5])
            update(h_b, cst[1], 1, bk[5]); update(h_b, cst[0], 1, bk[5])
            if b == 0 and g == 0:
                tap("h0f", h_f[:], [128, 512]); tap("h0b", h_b[:], [128, 512])
            for c in range(15, -1, -1):
                if c > 0:
                    prep(hxT, c - 1, lat[c - 1], True)
                P.I("pool", "tensor_copy", out=hin_b[c][:], in_=h_b[:])
                update(h_b, lat[c], 1, bk[4 + c % 2])
            stop("ssd_s1")
            P.release(m1)

            zs_r = Ring(P, "zs", 2, [128, 512]); CBmF_r = Ring(P, "CBmF", 2, [128, 128]); CBmB_r = Ring(P, "CBmB", 2, [128, 128])
            yoffF_r = Ring(P, "yoffF", 2, [128, 512], BF16); yoffB_r = Ring(P, "yoffB", 2, [128, 512], BF16); xskip_r = Ring(P, "xskip", 2, [128, 512], BF16)
            arepm_r = Ring(P, "arepm", 2, [128, 8, 128]); xdt_r = Ring(P, "xdt", 4, [128, 512], BF16); Lt_r = Ring(P, "Lt", 2, [128, 512])
            Mt_r = Ring(P, "Mt", 8, [128, 4, 128], BF16)
            yz_r = Ring(P, "yz", 2, [128, 512]); junk = P.sb("junk", [128, 512], BF16)
            ssq_r = Ring(P, "ssq", 2, [128, 1]); sd_r = Ring(P, "sd", 2, [128, 1]); rstd_r = Ring(P, "rstd", 2, [128, 1])
            yn_r = Ring(P, "yn", 2, [128, 512], BF16); ynT = P.sb("ynT", [128, 4, 512], BF16)

            def mainA(c):
                st = lat[c]
                A = dict(zs=zs_r.get(), CBm=[CBmF_r.get(), CBmB_r.get()], ecol=sm_r["ecol"].get(), xskip=xskip_r.get(), xdt=[], Mt=[])
                for kc in range(8):
                    P.I("pe", "matmul", out=bk[0][:], lhsT=hxT[:, kc, 2 + c * 128:2 + (c + 1) * 128], rhs=wZ[kc][:], start=(kc == 0), stop=(kc == 7))
                P.I("act", "activation", out=A["zs"][:], in_=bk[0][:], func=AF.Silu)
                P.I("pe", "matmul", out=bk[1][:, 0:128], lhsT=st["BT"][:], rhs=st["CT"][:], start=True, stop=True)
                P.I("pe", "matmul", out=bk[1][:, 128:136], lhsT=tri[:, TRI_LE, :], rhs=st["a"][:, 0:8], start=True, stop=True)
                P.I("pe", "matmul", out=bk[1][:, 136:144], lhsT=tri[:, TRI_GE, :], rhs=st["a"][:, 8:16], start=True, stop=True)
                P.I("dve", "tensor_tensor", out=A["CBm"][0][:], in0=bk[1][:, 0:128], in1=tri[:, TRI_LE, :], op=ALU.mult)
                P.I("dve", "tensor_tensor", out=A["CBm"][1][:], in0=bk[1][:, 0:128], in1=tri[:, TRI_GE, :], op=ALU.mult)
                P.I("act", "activation", out=A["ecol"][:], in_=bk[1][:, 128:144], func=AF.Exp)
                P.I("pool", "tensor_tensor", out=hv(A["xskip"][:]), in0=hv(st["xs"][:]), in1=dskip[:, g * 8:(g + 1) * 8].unsq(2).bcast([128, 8, 64]), op=ALU.mult)
                for dirn in range(2):
                    a_d = st["a"][:, dirn * 8:(dirn + 1) * 8]
                    arepm = arepm_r.get(); xdt = xdt_r.get()
                    A["xdt"].append(xdt)
                    P.I("dve", "tensor_tensor", out=arepm[:], in0=a_d.unsq(2).bcast([128, 8, 128]),
                        in1=tri[:, TRI_GT if dirn == 0 else TRI_LT, :].unsq(1).bcast([128, 8, 128]), op=ALU.mult)
                    P.I("pool", "tensor_tensor", out=hv(xdt[:]), in0=hv(st["xs"][:]), in1=st["dt"][:, dirn * 8:(dirn + 1) * 8].unsq(2).bcast([128, 8, 64]), op=ALU.mult)
                    for hb in range(2):
                        bankD = bk[5 + hb]
                        Lt = Lt_r.get(); Mt = Mt_r.get()
                        A["Mt"].append(Mt)
                        for h in range(4):
                            P.I("pe", "matmul", out=bankD[:, h * 128:(h + 1) * 128], lhsT=arepm[:, hb * 4 + h, :], rhs=tri[:, TRI_LE if dirn == 0 else TRI_GE, :], start=True, stop=True)
                        P.I("act", "activation", out=Lt[:], in_=bankD[:], func=AF.Exp)
                        P.I("pool", "tensor_tensor", out=Mt[:], in0=Lt[:].rearrange("p (h q) -> p h q", h=4), in1=A["CBm"][dirn][:].unsq(1).bcast([128, 4, 128]), op=ALU.mult)
                return A

            def mainB(c, A, hinf):
                st = lat[c]
                yoff = [yoffF_r.get(), yoffB_r.get()]; yz = yz_r.get(); ssq = ssq_r.get(); sd = sd_r.get(); rstd = rstd_r.get(); yn = yn_r.get()
                P.I("pe", "matmul", out=bk[2][:], lhsT=st["CT"][:], rhs=hinf[:], start=True, stop=True)
                P.I("pe", "matmul", out=bk[3][:], lhsT=st["CT"][:], rhs=hin_b[c][:], start=True, stop=True)
                for dirn in range(2):
                    P.I("dve", "tensor_tensor", out=hv(yoff[dirn][:]), in0=hv(bk[2 + dirn][:]), in1=A["ecol"][:, dirn * 8:(dirn + 1) * 8].unsq(2).bcast([128, 8, 64]), op=ALU.mult)
                P.I("pe", "matmul", out=bk[4][:], lhsT=ident_b[:], rhs=A["xskip"][:], start=True, stop=False)
                P.I("pe", "matmul", out=bk[4][:], lhsT=ident_b[:], rhs=yoff[0][:], start=False, stop=False)
                P.I("pe", "matmul", out=bk[4][:], lhsT=ident_b[:], rhs=yoff[1][:], start=False, stop=False)
                for dirn in range(2):
                    for hb in range(2):
                        Mt = A["Mt"][dirn * 2 + hb]; xdt = A["xdt"][dirn]
                        for h in range(4):
                            hh = hb * 4 + h
                            P.I("pe", "matmul", out=bk[4][:, hh * 64:(hh + 1) * 64], lhsT=Mt[:, h, :], rhs=xdt[:, hh * 64:(hh + 1) * 64], start=False, stop=(dirn == 1 and hh == 7))
                P.I("dve", "tensor_tensor", out=yz[:], in0=bk[4][:], in1=A["zs"][:], op=ALU.mult)
                P.I("act", "activation", out=junk[:], in_=yz[:], func=AF.Square, accum_out=ssq[:])
                P.I("act", "activation", out=sd[:], in_=ssq[:], func=AF.Sqrt, scale=1.0 / 512.0, bias=eps6[:, 0:1])
                P.I("dve", "reciprocal", out=rstd[:], in_=sd[:])
                P.I("dve", "scalar_tensor_tensor", out=yn[:], in0=yz[:], scalar=rstd[:, 0:1], in1=ssdg[:, g * 512:(g + 1) * 512], op0=ALU.mult, op1=ALU.mult)
                for fc in range(4):
                    P.I("pe", "matmul", out=bk[7][:, fc * 128:(fc + 1) * 128], lhsT=yn[:, fc * 128:(fc + 1) * 128], rhs=ident_b[:], start=True, stop=True)
                P.I("act", "activation", out=ynT[:, :, (c % 4) * 128:(c % 4 + 1) * 128], in_=bk[7][:].rearrange("p (f t) -> p f t", f=4), func=AF.Identity)
                if c % 4 == 3:
                    for fc in range(4):
                        P.dma(cat[g * 4 + fc, :, (c // 4) * 512:(c // 4 + 1) * 512], ynT[:, fc, :])

            if b == 0 and g == 0:
                tap("xs0", lat[0]["xs"][:], [128, 512], BF16)
            A_next = mainA(0)
            for c in range(16):
                hf16 = hin_f.get()
                P.I("pool", "tensor_copy", out=hf16[:], in_=h_f[:])
                update(h_f, lat[c], 0, bk[7])
                A_cur = A_next
                if c + 1 < 16:
                    A_next = mainA(c + 1)
                mainB(c, A_cur, hf16)
            P.release(mp)

        for g in range(0 if "ssd" not in skip else 2, 2):
            ssd_pass(g)
            if b == 0 and g == 0:
                stop("ssd0")
        stop("ssd")

        ma = P.mark()
        cosT = P.sb("cosT", [128, T]); sinT = P.sb("sinT", [128, T])
        P.dma(cosT[:], D.c_cos); P.dma(sinT[:], D.c_sin)
        wsets = [dict(wq=P.sb(f"wq{i}", [128, 8, 256], BF16), wkd=P.sb(f"wkd{i}", [128, 8, 128], BF16), wv=P.sb(f"wv{i}", [128, 8, 64], BF16)) for i in range(2)]
        asets = [dict(KT=P.sb(f"KT{i}", [128, TC + T], BF16), VA=P.sb(f"VA{i}", [128, 18, 128], BF16), VB=P.sb(f"VB{i}", [128, 18, 128], BF16),
                      qT=P.sb(f"qT{i}", [128, 2, T], BF16)) for i in range(2)]
        for a_ in asets:
            P.I("pool", "memset", ap=a_["VA"][:], constant=1.0); P.I("pool", "memset", ap=a_["VB"][:], constant=1.0)
        nr_r = {n: Ring(P, n, 2, [128, 512], dt_) for n, dt_ in (("kgb", BF16), ("sqb", BF16), ("sdn", F32), ("rsn", F32), ("t1", F32), ("t2", F32))}
        PT = Ring(P, "PT", 3, [128, 1024], BF16); oTr = Ring(P, "oT", 2, [128, 2, 512], BF16); rec = P.sb("rec", [128, 512])

        def load_w(g):
            W = wsets[g % 2]
            for kc in range(8):
                st = stage.get()
                rows = slice(kc * 128, (kc + 1) * 128)
                P.dma(st[:, 0:256], D.w_in[rows, OFF_Q + g * 256:OFF_Q + (g + 1) * 256])
                P.dma(st[:, 256:320], D.w_in[rows, OFF_K + g * 64:OFF_K + (g + 1) * 64])
                P.dma(st[:, 320:384], D.w_in[rows, OFF_V + g * 64:OFF_V + (g + 1) * 64])
                P.I("pool", "tensor_copy", out=W["wq"][:, kc, :], in_=st[:, 0:256])
                P.I("pool", "tensor_copy", out=W["wkd"][:, kc, 0:64], in_=st[:, 256:320])
                P.I("pool", "tensor_copy", out=W["wkd"][:, kc, 64:128], in_=st[:, 256:320])
                P.I("pool", "tensor_copy", out=W["wv"][:, kc, :], in_=st[:, 320:384])

        def norm_rope(gvec, dst, n, tok0, bp, bs):
            kgb, sqb, sdn, rsn, t1, t2 = (nr_r[n_].get() for n_ in ("kgb", "sqb", "sdn", "rsn", "t1", "t2"))
            yield
            P.I("act", "activation", out=kgb[:, 0:n], in_=bp[:, 0:n], func=AF.Identity, scale=gvec[:, 0:1])
            P.I("act", "activation", out=sqb[:, 0:n], in_=bp[:, 0:n], func=AF.Square)
            yield
            P.I("pe", "matmul", out=bs[:, 0:n], lhsT=bones_b[:], rhs=sqb[:, 0:n], start=True, stop=True)
            if tok0 is not None:
                P.I("pe", "matmul", out=bp[:, 0:n], lhsT=prot_b[:], rhs=kgb[:, 0:n], start=True, stop=True)
                P.I("pool", "tensor_tensor", out=t1[:, 0:n], in0=kgb[:, 0:n], in1=cosT[:, tok0:tok0 + n], op=ALU.mult)
            yield
            P.I("act", "activation", out=sdn[:, 0:n], in_=bs[:, 0:n], func=AF.Sqrt, scale=1.0 / 64.0, bias=eps6[:, 0:1])
            if tok0 is not None:
                P.I("dve", "tensor_tensor", out=t2[:, 0:n], in0=bp[:, 0:n], in1=sinT[:, tok0:tok0 + n], op=ALU.mult)
            yield
            P.I("dve", "reciprocal", out=rsn[:, 0:n], in_=sdn[:, 0:n])
            if tok0 is None:
                yield
                P.I("dve", "tensor_tensor", out=dst, in0=kgb[:, 0:n], in1=rsn[:, 0:n], op=ALU.mult)
            else:
                P.I("pool", "tensor_tensor", out=t1[:, 0:n], in0=t1[:, 0:n], in1=t2[:, 0:n], op=ALU.add)
                yield
                P.I("dve", "tensor_tensor", out=dst, in0=t1[:, 0:n], in1=rsn[:, 0:n], op=ALU.mult)
            yield

        def qkv_units(g):
            W = wsets[g % 2]; S_ = asets[g % 2]
            ucnt = [0]

            def banks():
                ucnt[0] += 1
                return (bk[0], bk[1]) if ucnt[0] % 2 else (bk[2], bk[3])

            def vunit(hT, col0, t0, nt):
                _, bs = banks()
                for tt in range(nt):
                    for kc in range(8):
                        P.I("pe", "matmul", out=bs[:, tt * 64:(tt + 1) * 64], lhsT=hT[:, kc, col0 + tt * 128:col0 + (tt + 1) * 128], rhs=W["wv"][:, kc, :], start=(kc == 0), stop=(kc == 7))
                src = bs[:, 0:nt * 64].rearrange("p (t d) -> p t d", t=nt)
                yield
                P.I("act", "activation", out=S_["VA"][:, t0:t0 + nt, 0:64], in_=src, func=AF.Identity)
                P.I("dve", "tensor_copy", out=S_["VB"][:, t0:t0 + nt, 64:128], in_=src)

            bp, bs = banks()
            for kc in range(8):
                P.I("pe", "matmul", out=bp[:, 0:TC], lhsT=W["wkd"][:, kc, :], rhs=hcT[:, kc, 2:2 + TC], start=(kc == 0), stop=(kc == 7))
            yield from norm_rope(gk, S_["KT"][:, 0:TC], TC, None, bp, bs)
            yield from vunit(hcT, 2, 0, 2)
            yield
            for blk in range(4):
                tok0 = blk * 512
                bp, bs = banks()
                for kc in range(8):
                    P.I("pe", "matmul", out=bp[:], lhsT=W["wkd"][:, kc, :], rhs=hxT[:, kc, 2 + tok0:2 + tok0 + 512], start=(kc == 0), stop=(kc == 7))
                yield from norm_rope(gk, S_["KT"][:, TC + tok0:TC + tok0 + 512], 512, tok0, bp, bs)
                yield from vunit(hxT, 2 + tok0, 2 + blk * 4, 4)
                yield
                for j in range(2):
                    bp, bs = banks()
                    for kc in range(8):
                        P.I("pe", "matmul", out=bp[:], lhsT=W["wq"][:, kc, j * 128:(j + 1) * 128], rhs=hxT[:, kc, 2 + tok0:2 + tok0 + 512], start=(kc == 0), stop=(kc == 7))
                    yield from norm_rope(gq, S_["qT"][:, j, tok0:tok0 + 512], 512, tok0, bp, bs)

        sbanks = [bk[4], bk[5], bk[6], bk[7]]; obanks4 = [bk[0], bk[1], bk[2], bk[3]]
        its = [(qb, j, kt) for qb in range(4) for j in range(2) for kt in range(18)]

        def main_loop(g, filler):
            S_ = asets[g % 2]
            KT, VA, VB, qT = S_["KT"], S_["VA"], S_["VB"], S_["qT"]

            def s_mm(i):
                qb, j, kt = its[i]
                for half in range(2):
                    rows = slice(half * 64, half * 64 + 64)
                    P.I("pe", "matmul", out=spair[i % 2][:, half * 512:(half + 1) * 512], lhsT=KT[rows, kt * 128:(kt + 1) * 128], rhs=qT[rows, j, qb * 512:(qb + 1) * 512], start=True, stop=True)

            s_mm(0)
            oTb = None
            for i, (qb, j, kt) in enumerate(its):
                if i + 1 < len(its):
                    s_mm(i + 1)
                if j == 0 and kt == 0:
                    oTb = oTr.get()
                ptp = PT.get()
                obanks = obanks4[((i // 18) % 2) * 2:((i // 18) % 2) * 2 + 2]
                P.I("act", "activation", out=ptp[:], in_=spair[i % 2][:], func=AF.Exp, scale=0.125)
                for half in range(2):
                    P.I("pe", "matmul", out=obanks[half][:], lhsT=(VA if half == 0 else VB)[:, kt, :], rhs=ptp[:, half * 512:(half + 1) * 512], start=(kt == 0), stop=(kt == 17))
                if kt == 17:
                    P.I("dve", "reciprocal", out=rec[0:64, :], in_=obanks[0][64:128, :])
                    P.I("dve", "tensor_tensor", out=oTb[0:64, j, :], in0=obanks[0][0:64, :], in1=rec[0:64, :], op=ALU.mult)
                    P.I("dve", "reciprocal", out=rec[64:128, :], in_=obanks[1][0:64, :])
                    P.I("dve", "tensor_tensor", out=oTb[64:128, j, :], in0=obanks[1][64:128, :], in1=rec[64:128, :], op=ALU.mult)
                    if j == 1:
                        for jj in range(2):
                            P.dma(cat[8 + g * 2 + jj, :, qb * 512:(qb + 1) * 512], oTb[:, jj, :])
                if filler is not None:
                    next(filler, None)
            if filler is not None:
                for _ in filler:
                    pass

        load_w(0)
        for _ in qkv_units(0):
            pass
        load_w(1)
        for g in range(4):
            if g + 2 < 4:
                load_w(g + 2)
            main_loop(g, None)
            if g + 1 < 4:
                for _ in qkv_units(g + 1):
                    pass
        P.release(ma)
        stop("attn")
        P.release(mb)

        acc = [P.sb(f"acc{i}", [128, 1024]) for i in range(16)]
        hT = [P.sb(f"hT{i}", [128, 8, 512], BF16) for i in range(4)]
        comb = P.sb("comb", [128, 16, 16])
        sml = {n: P.sb("r_" + n, [128, w]) for n, w in (("lgt", 20), ("m", 1), ("negm", 1), ("oh", 4), ("eg", 4), ("se", 1), ("gval", 1), ("prod", 16),
                                                          ("esel", 4), ("m1", 1), ("oh1", 4), ("msk", 4), ("m2", 1), ("oh2", 4), ("d12", 1), ("e21", 1),
                                                          ("den", 1), ("w1", 1), ("w2", 1), ("tq", 4), ("wg", 4), ("ohg", 4), ("s1", 1), ("nm", 1),
                                                          ("ssq", 1), ("sd", 1), ("rstd", 1))}
        junkf = P.sb("junkf", [128, 1024], BF16)
        mo2 = P.mark()
        woH = [P.sb(f"woH{i}", [128, 1024], BF16) for i in range(8)]; catb_r = Ring(P, "catb", 2, [128, 8, 512], BF16)
        lng = P.sb("lng", [128, 1024]); lnb = P.sb("lnb", [128, 1024]); hf = P.sb("hf", [128, 8, 128])
        P.dma(lng[:], D.ln1_g.partition_broadcast(128)); P.dma(lnb[:], D.ln1_b.partition_broadcast(128))
        P.I("dve", "tensor_scalar", out=lnb[:], in0=lnb[:], scalar1=ALPHA, scalar2=None, op0=ALU.mult)

        def layer_norm_tile(a, g_t, b_t, post_scale):
            P.I("dve", "tensor_reduce", out=sml["s1"][:], in_=a[:], axis=AX.X, op=ALU.add)
            P.I("dve", "tensor_scalar", out=sml["nm"][:], in0=sml["s1"][:], scalar1=-1.0 / 1024.0, scalar2=None, op0=ALU.mult)
            P.I("act", "activation", out=a[:], in_=a[:], func=AF.Identity, bias=sml["nm"][:, 0:1])
            P.I("act", "activation", out=junkf[:], in_=a[:], func=AF.Square, accum_out=sml["ssq"][:])
            P.I("act", "activation", out=sml["sd"][:], in_=sml["ssq"][:], func=AF.Sqrt, scale=1.0 / 1024.0, bias=eps5[:, 0:1])
            P.I("dve", "reciprocal", out=sml["rstd"][:], in_=sml["sd"][:])
            if post_scale != 1.0:
                P.I("dve", "tensor_scalar", out=sml["rstd"][:], in0=sml["rstd"][:], scalar1=post_scale, scalar2=None, op0=ALU.mult)
            P.I("dve", "scalar_tensor_tensor", out=a[:], in0=a[:], scalar=sml["rstd"][:, 0:1], in1=g_t[:], op0=ALU.mult, op1=ALU.mult)
            P.I("dve", "tensor_tensor", out=a[:], in0=a[:], in1=b_t[:], op=ALU.add)

        def router(ti):
            S = sml
            g4 = lambda v: v.rearrange("p (g k) -> p g k", g=4)
            for c in range(8):
                P.I("pe", "matmul", out=bk[2][:, 0:20], lhsT=hf[:, c, :], rhs=wr[:, c, :], start=(c == 0), stop=(c == 7))
            P.I("dve", "tensor_tensor", out=S["lgt"][:], in0=bk[2][:, 0:20], in1=br[:], op=ALU.add)
            P.I("dve", "tensor_reduce", out=S["m"][:], in_=S["lgt"][:, 0:4], axis=AX.X, op=ALU.max)
            P.I("dve", "tensor_scalar", out=S["oh"][:], in0=S["lgt"][:, 0:4], scalar1=S["m"][:, 0:1], scalar2=None, op0=ALU.is_equal)
            P.I("dve", "tensor_scalar", out=S["negm"][:], in0=S["m"][:], scalar1=-1.0, scalar2=None, op0=ALU.mult)
            P.I("act", "activation", out=S["eg"][:], in_=S["lgt"][:, 0:4], func=AF.Exp, bias=S["negm"][:, 0:1], accum_out=S["se"][:])
            P.I("dve", "reciprocal", out=S["gval"][:], in_=S["se"][:])
            P.I("dve", "tensor_tensor", out=g4(S["prod"][:]), in0=g4(S["lgt"][:, 4:20]), in1=S["oh"][:].unsq(2).bcast([128, 4, 4]), op=ALU.mult)
            P.I("dve", "tensor_reduce", out=S["esel"][:], in_=S["prod"][:].rearrange("p (g k) -> p k g", g=4), axis=AX.X, op=ALU.add)
            P.I("dve", "tensor_reduce", out=S["m1"][:], in_=S["esel"][:], axis=AX.X, op=ALU.max)
            P.I("dve", "tensor_scalar", out=S["oh1"][:], in0=S["esel"][:], scalar1=S["m1"][:, 0:1], scalar2=None, op0=ALU.is_equal)
            P.I("dve", "scalar_tensor_tensor", out=S["msk"][:], in0=S["oh1"][:], scalar=-1e30, in1=S["esel"][:], op0=ALU.mult, op1=ALU.add)
            P.I("dve", "tensor_reduce", out=S["m2"][:], in_=S["msk"][:], axis=AX.X, op=ALU.max)
            P.I("dve", "tensor_scalar", out=S["oh2"][:], in0=S["msk"][:], scalar1=S["m2"][:, 0:1], scalar2=None, op0=ALU.is_equal)
            P.I("dve", "tensor_tensor", out=S["d12"][:], in0=S["m2"][:], in1=S["m1"][:], op=ALU.subtract)
            P.I("act", "activation", out=S["e21"][:], in_=S["d12"][:], func=AF.Exp)
            P.I("dve", "tensor_scalar", out=S["den"][:], in0=S["e21"][:], scalar1=1.0, scalar2=None, op0=ALU.add)
            P.I("dve", "reciprocal", out=S["w1"][:], in_=S["den"][:])
            P.I("dve", "tensor_tensor", out=S["w2"][:], in0=S["e21"][:], in1=S["w1"][:], op=ALU.mult)
            P.I("dve", "tensor_scalar", out=S["tq"][:], in0=S["oh1"][:], scalar1=S["w1"][:, 0:1], scalar2=None, op0=ALU.mult)
            P.I("dve", "scalar_tensor_tensor", out=S["wg"][:], in0=S["oh2"][:], scalar=S["w2"][:, 0:1], in1=S["tq"][:], op0=ALU.mult, op1=ALU.add)
            P.I("dve", "tensor_scalar", out=S["ohg"][:], in0=S["oh"][:], scalar1=S["gval"][:, 0:1], scalar2=None, op0=ALU.mult)
            P.I("dve", "tensor_tensor", out=g4(comb[:, ti, :]), in0=S["ohg"][:].unsq(2).bcast([128, 4, 4]), in1=S["wg"][:].unsq(1).bcast([128, 4, 4]), op=ALU.mult)

        def ln1_tile(ti):
            layer_norm_tile(acc[ti], lng, lnb, ALPHA)
            for c in range(8):
                P.I("pe", "transpose", out=bk[c // 4][:, (c % 4) * 128:(c % 4 + 1) * 128], in_=acc[ti][:, c * 128:(c + 1) * 128], identity=ident_f[:])
            for c in range(8):
                src = bk[c // 4][:, (c % 4) * 128:(c % 4 + 1) * 128]
                if c % 2 == 0:
                    P.I("act", "activation", out=hf[:, c, :], in_=src, func=AF.Identity, scale=modT[:, 32 + c, b:b + 1], bias=modT[:, 24 + c, b:b + 1])
                else:
                    P.I("dve", "tensor_scalar", out=hf[:, c, :], in0=src, scalar1=modT[:, 32 + c, b:b + 1], scalar2=modT[:, 24 + c, b:b + 1], op0=ALU.mult, op1=ALU.add)
            P.I("pool", "tensor_copy", out=hT[ti // 4][:, :, (ti % 4) * 128:(ti % 4 + 1) * 128], in_=hf[:])
            router(ti)

        for kh in range(2):
            for kc in range(8):
                st = stage.get()
                P.dma(st[:, 0:1024], D.w_out[(kh * 8 + kc) * 128:(kh * 8 + kc + 1) * 128, :])
                P.I("pool", "tensor_tensor", out=woH[kc][:], in0=st[:, 0:1024], in1=gA[:], op=ALU.mult)
            for blk in range(4):
                catb = catb_r.get()
                P.dma(catb[:], cat[kh * 8:(kh + 1) * 8, :, blk * 512:(blk + 1) * 512].rearrange("k p t -> p k t"))
                for tt in range(4):
                    ti = blk * 4 + tt
                    if kh == 0:
                        xt_ = xt.get()
                        P.dma(xt_[:], D.x[b, ti * 128:(ti + 1) * 128, :])
                    for half in range(2):
                        bank = bk[3 + (ti * 2 + half) % 5]
                        hs = slice(half * 512, (half + 1) * 512)
                        for kc in range(8):
                            P.I("pe", "matmul", out=bank[:], lhsT=catb[:, kc, tt * 128:(tt + 1) * 128], rhs=woH[kc][:, hs], start=(kc == 0), stop=(kc == 7))
                        if kh == 0:
                            P.I("dve", "scalar_tensor_tensor", out=acc[ti][:, hs], in0=xt_[:, hs], scalar=ALPHA, in1=bank[:], op0=ALU.mult, op1=ALU.add)
                        else:
                            P.I("dve", "tensor_tensor", out=acc[ti][:, hs], in0=bank[:], in1=acc[ti][:, hs], op=ALU.add)
                    if kh == 1:
                        ln1_tile(ti)
        if b == 0:
            tap("x1a0", acc[0][:], [128, 1024]); tap("comb", comb[:].rearrange("p t e -> p (t e)"), [128, 256])
            tap("hT0", hT[0][:].rearrange("p k t -> p (k t)"), [128, 4096], BF16)
        stop("ln1")
        P.release(mo2)

        lng2 = P.sb("lng2", [128, 1024]); P.dma(lng2[:], D.ln2_g.partition_broadcast(128))
        lnb2 = P.sb("lnb2", [128, 1024]); P.dma(lnb2[:], D.ln2_b.partition_broadcast(128))
        wgb = Ring(P, "wgb", 2, [128, 8, 256], BF16); wub = Ring(P, "wub", 2, [128, 8, 256], BF16); wdb = Ring(P, "wdb", 2, [128, 2, 1024], BF16)
        sgr = Ring(P, "sg", 2, [128, 2, 512], BF16); hidr = Ring(P, "hid", 2, [128, 2, 512], BF16)
        def load_expert(e):
            st = stage.get(); wg_ = wgb.get()
            P.dma(st[:].rearrange("p (k f) -> p k f", k=8), D.w_gate[e].rearrange("(k p) f -> p k f", p=128))
            P.I("pool", "tensor_copy", out=wg_[:].rearrange("p k f -> p (k f)"), in_=st[:])
            st = stage.get(); wu_ = wub.get()
            P.dma(st[:].rearrange("p (k f) -> p k f", k=8), D.w_up[e].rearrange("(k p) f -> p k f", p=128))
            P.I("pool", "tensor_copy", out=wu_[:].rearrange("p k f -> p (k f)"), in_=st[:])
            st = stage.get(); wd_ = wdb.get()
            P.dma(st[:].rearrange("p (k d) -> p k d", k=2), D.w_down[e].rearrange("(k p) d -> p k d", p=128))
            for k in range(2):
                P.I("pool", "tensor_tensor", out=wd_[:, k, :], in0=st[:, k * 1024:(k + 1) * 1024], in1=gF[:], op=ALU.mult)
            return wg_, wu_, wd_

        wts = {0: load_expert(0)}
        items = [(e, blk) for e in range(16) for blk in range(4)]
        hids = {}

        def gate_up(i):
            e, blk = items[i]
            if blk == 1 and e + 1 < 16:
                wts[e + 1] = load_expert(e + 1)
            wg_, wu_, _ = wts[e]
            for fc in range(2):
                for kc in range(8):
                    P.I("pe", "matmul", out=bk[2 * fc][:], lhsT=wg_[:, kc, fc * 128:(fc + 1) * 128], rhs=hT[blk][:, kc, :], start=(kc == 0), stop=(kc == 7))
                for kc in range(8):
                    P.I("pe", "matmul", out=bk[2 * fc + 1][:], lhsT=wu_[:, kc, fc * 128:(fc + 1) * 128], rhs=hT[blk][:, kc, :], start=(kc == 0), stop=(kc == 7))
            sg = sgr.get(); hid = hidr.get()
            for fc in range(2):
                P.I("act", "activation", out=sg[:, fc, :], in_=bk[2 * fc][:], func=AF.Silu)
                P.I("dve", "tensor_tensor", out=hid[:, fc, :], in0=bk[2 * fc + 1][:], in1=sg[:, fc, :], op=ALU.mult)
            hids[i] = hid

        def down(i):
            e, blk = items[i]
            hid = hids.pop(i); wd_ = wts[e][2]
            for tt in range(4):
                ti = blk * 4 + tt
                for half in range(2):
                    bank = bk[4 + (tt * 2 + half) % 4]
                    hs = slice(half * 512, (half + 1) * 512)
                    for fc in range(2):
                        P.I("pe", "matmul", out=bank[:], lhsT=hid[:, fc, tt * 128:(tt + 1) * 128], rhs=wd_[:, fc, hs], start=(fc == 0), stop=(fc == 1))
                    P.I("dve", "scalar_tensor_tensor", out=acc[ti][:, hs], in0=bank[:], scalar=comb[:, ti, e:e + 1], in1=acc[ti][:, hs], op0=ALU.mult, op1=ALU.add)

        gate_up(0)
        for i in range(len(items)):
            if i + 1 < len(items):
                gate_up(i + 1)
            down(i)
        if b == 0:
            tap("pre2", acc[0][:], [128, 1024])
        for ti in range(16):
            layer_norm_tile(acc[ti], lng2, lnb2, 1.0)
            P.dma(ybuf[b, ti * 128:(ti + 1) * 128, :], acc[ti][:])
        stop("moe")
        P.release(mb)

    P.barrier()
    return nc, P


def finish(nc, P):
    P.barrier()
    P.build()
    return nc


def _const_tables():
    i = np.arange(128)
    le = (i[:, None] <= i[None, :]).astype(np.float32)
    ge = (i[:, None] >= i[None, :]).astype(np.float32)
    gt = (i[:, None] > i[None, :]).astype(np.float32)
    lt = (i[:, None] < i[None, :]).astype(np.float32)
    tri = np.stack([le, ge, gt, lt, np.ones((128, 128), np.float32)])
    prot = np.zeros((128, 128), np.float32)
    for j in range(64):
        prot[2 * j + 1, 2 * j] = -1.0
        prot[2 * j, 2 * j + 1] = 1.0
    bones = (i[:, None] // 64 == i[None, :] // 64).astype(np.float32)
    t = np.arange(T)
    rows = (t // 64).astype(np.float32)
    cols = (t % 64).astype(np.float32)
    inv_freq = np.power(np.float32(10000.0), -np.arange(0, 32, 2, dtype=np.float32) / np.float32(32)).astype(np.float32)
    ang = np.concatenate([rows[:, None] * inv_freq, cols[:, None] * inv_freq], -1).astype(np.float32)
    pair = (i % 64) // 2
    return {"c_ident": np.eye(128, dtype=np.float32), "c_tri": tri, "c_prot": prot, "c_bones": bones,
            "c_cos": np.ascontiguousarray(np.cos(ang)[:, pair].T.astype(np.float32)),
            "c_sin": np.ascontiguousarray(np.sin(ang)[:, pair].T.astype(np.float32))}


def _core_inputs(d, core, consts):
    b0 = core * NB
    f = lambda a: np.ascontiguousarray(np.asarray(a, dtype=np.float32))
    m = {"x": d["x"][b0:b0 + NB], "ctx": d["ctx"][b0:b0 + NB],
         "crow": np.concatenate([np.asarray(d["c"])[b0:b0 + NB], np.asarray(d["c_ctx"])[None]], 0),
         "w_mod": d["w_mod"][0], "b_mod": d["b_mod"], "w_in": d["w_in"][0], "conv_w": d["conv_w"][0], "conv_b": d["conv_b"],
         "dt_bias": np.asarray(d["dt_bias"]).reshape(1, 32), "a_log": np.asarray(d["a_log"]).reshape(1, 32), "d_skip": d["d_skip"],
         "ssd_norm_g": d["ssd_norm_g"], "q_norm_g": np.asarray(d["q_norm_g"]).reshape(64, 1), "k_norm_g": np.asarray(d["k_norm_g"]).reshape(64, 1),
         "w_out": d["w_out"][0], "ln1_g": d["ln1_g"], "ln1_b": d["ln1_b"], "ln2_g": d["ln2_g"], "ln2_b": d["ln2_b"],
         "w_r": np.concatenate([np.asarray(d["w_rg"])[0], np.asarray(d["w_re"])[0]], 1),
         "b_r": np.concatenate([np.asarray(d["b_rg"]), np.asarray(d["b_re"])], 1),
         "w_gate": d["w_gate"][0], "w_up": d["w_up"][0], "w_down": d["w_down"][0]}
    m.update(consts)
    return {k: f(v) for k, v in m.items()}


def kernel(**inputs):
    n_cores = 8
    nc, P = build_program()
    finish(nc, P)
    consts = _const_tables()
    shared = None
    in_maps = []
    for core in range(n_cores):
        ci = _core_inputs(inputs, core, consts)
        if shared is None:
            shared = ci
        else:
            for k in ci:
                if k not in ("x", "ctx", "crow"):
                    ci[k] = shared[k]
        in_maps.append({k: ci[k] for k in used_inputs})
    res = run_bass_kernel_spmd(nc, in_maps, core_ids=list(range(n_cores)))
    return np.concatenate([np.asarray(r["y"]) for r in res.results], axis=0).astype(np.float32)
```
